# Optimizing a Trainium2 kernel written in Bass

```python
import jax, jax.numpy as jnp
from jax import lax
import numpy as np

D_MODEL = 1024
BATCH = 16
SEQ = 2048
DEPTH = 1

HEAD_DIM = 64
A_HEADS = 8
A_WIDTH = A_HEADS * HEAD_DIM
CHUNK = 128
B_HEADS = 8
B_KV_HEADS = 2
B_GROUP = B_HEADS // B_KV_HEADS
B_WIDTH = B_HEADS * HEAD_DIM
KV_WIDTH = B_KV_HEADS * HEAD_DIM
D_MIX = A_WIDTH + B_WIDTH
IN_WIDTH = 2 * A_WIDTH + B_WIDTH + 2 * KV_WIDTH
Q_BLOCK = 128
GRID_W = 64
ROPE_BASE = 10000.0
N_EXPERTS = 16
CAPACITY_FACTOR = 2
EXPERT_FF = 1024
EPS = 1e-6

kernel_name = "hybrid_gmlp_axialgqa_ecmoe_encoder"


def rms_norm(x, g):
    xf = x.astype(jnp.float32)
    y = xf * lax.rsqrt(jnp.mean(xf * xf, axis=-1, keepdims=True) + EPS)
    return (y * g.astype(jnp.float32)).astype(x.dtype)


def rope_1d(x, pos):
    half = x.shape[-1] // 2
    inv_freq = ROPE_BASE ** (-jnp.arange(half, dtype=jnp.float32) / half)
    ang = pos.astype(jnp.float32)[:, None] * inv_freq[None, :]
    cos = jnp.cos(ang)[:, None, :]
    sin = jnp.sin(ang)[:, None, :]
    xf = x.astype(jnp.float32)
    x1, x2 = xf[..., :half], xf[..., half:]
    return jnp.concatenate([x1 * cos - x2 * sin, x2 * cos + x1 * sin], axis=-1).astype(x.dtype)


def axial_rope(x, row_pos, col_pos):
    d_axis = x.shape[-1] // 2
    return jnp.concatenate([rope_1d(x[..., :d_axis], row_pos),
                            rope_1d(x[..., d_axis:], col_pos)], axis=-1)


def chunked_spatial_gating(u, v, g_v, w_s, b_s):
    B, S, _ = u.shape
    n_chunks = S // CHUNK
    u = jax.nn.gelu(u).reshape(B, n_chunks, CHUNK, A_HEADS, HEAD_DIM)
    v = rms_norm(jax.nn.gelu(v).reshape(B, S, A_HEADS, HEAD_DIM), g_v)
    v = v.reshape(B, n_chunks, CHUNK, A_HEADS, HEAD_DIM)
    gate = jnp.einsum('hpq,bnqhd->bnphd', w_s, v) + b_s.T[None, None, :, :, None]
    return (u * gate).reshape(B, S, A_WIDTH)


def grid_attention(q, k, v, g_q, g_k):
    B, S, _ = q.shape
    rows = S // GRID_W
    row_pos = jnp.repeat(jnp.arange(rows, dtype=jnp.int32), GRID_W)
    col_pos = jnp.tile(jnp.arange(GRID_W, dtype=jnp.int32), rows)
    q = rms_norm(q.reshape(B, S, B_HEADS, HEAD_DIM), g_q)
    k = rms_norm(k.reshape(B, S, B_KV_HEADS, HEAD_DIM), g_k)
    v = v.reshape(B, S, B_KV_HEADS, HEAD_DIM)
    q = axial_rope(q, row_pos, col_pos)
    k = axial_rope(k, row_pos, col_pos)
    n_blocks = S // Q_BLOCK
    q = q.reshape(B, n_blocks, Q_BLOCK, B_KV_HEADS, B_GROUP, HEAD_DIM).transpose(1, 0, 2, 3, 4, 5)
    scale = HEAD_DIM ** -0.5

    def attend_block(qb):
        s = jnp.einsum('bqkgd,bskd->bkgqs', qb, k, preferred_element_type=jnp.float32) * scale
        p = jax.nn.softmax(s, axis=-1)
        return jnp.einsum('bkgqs,bskd->bqkgd', p.astype(v.dtype), v)

    o = lax.map(attend_block, q)
    return o.transpose(1, 0, 2, 3, 4, 5).reshape(B, S, B_WIDTH)


def expert_choice_ffn(h, w_router, w_gate, w_up, w_down):
    B, S, D = h.shape
    cap = CAPACITY_FACTOR * S // N_EXPERTS
    logits = jnp.einsum('bsd,de->bse', h, w_router, preferred_element_type=jnp.float32)
    affinity = jax.nn.softmax(logits, axis=-1)
    g, idx = lax.top_k(affinity.transpose(0, 2, 1), cap)
    b_idx = jnp.arange(B)[:, None, None]
    xs = h[b_idx, idx]
    a = jnp.einsum('becd,edf->becf', xs, w_gate)
    b = jnp.einsum('becd,edf->becf', xs, w_up)
    y = jnp.einsum('becf,efd->becd', jax.nn.silu(a) * b, w_down)
    y = y * g[..., None].astype(y.dtype)
    return jnp.zeros_like(h).at[b_idx, idx].add(y)


def setup_inputs(seed: int = 0) -> dict:
    key = jax.random.key(seed)
    ks = jax.random.split(key, 20)
    f32 = jnp.float32
    n = lambda k, shape, s: jax.random.normal(k, shape, f32) * s
    gain = lambda k, shape: 1.0 + 0.01 * jax.random.normal(k, shape, f32)
    return {
        "x": jax.random.normal(ks[0], (BATCH, SEQ, D_MODEL), f32),
        "norm_mix_g": gain(ks[1], (DEPTH, D_MODEL)),
        "w_in": n(ks[2], (DEPTH, D_MODEL, IN_WIDTH), D_MODEL ** -0.5),
        "gmlp_v_norm_g": gain(ks[3], (DEPTH, A_HEADS, HEAD_DIM)),
        "gmlp_w_s": n(ks[4], (DEPTH, A_HEADS, CHUNK, CHUNK), CHUNK ** -0.5),
        "gmlp_b_s": 1.0 + 0.1 * jax.random.normal(ks[5], (DEPTH, A_HEADS, CHUNK), f32),
        "q_norm_g": gain(ks[6], (DEPTH, HEAD_DIM)),
        "k_norm_g": gain(ks[7], (DEPTH, HEAD_DIM)),
        "group_norm_a_g": gain(ks[8], (DEPTH, A_WIDTH)),
        "group_norm_b_g": gain(ks[9], (DEPTH, B_WIDTH)),
        "w_out": n(ks[10], (DEPTH, D_MIX, D_MODEL), D_MIX ** -0.5),
        "norm_ffn_g": gain(ks[11], (DEPTH, D_MODEL)),
        "w_router": n(ks[12], (DEPTH, D_MODEL, N_EXPERTS), D_MODEL ** -0.5),
        "w_gate": n(ks[13], (DEPTH, N_EXPERTS, D_MODEL, EXPERT_FF), D_MODEL ** -0.5),
        "w_up": n(ks[14], (DEPTH, N_EXPERTS, D_MODEL, EXPERT_FF), D_MODEL ** -0.5),
        "w_down": n(ks[15], (DEPTH, N_EXPERTS, EXPERT_FF, D_MODEL), EXPERT_FF ** -0.5),
        "final_norm_g": gain(ks[16], (D_MODEL,)),
    }


def reference(x, norm_mix_g, w_in, gmlp_v_norm_g, gmlp_w_s, gmlp_b_s, q_norm_g, k_norm_g,
              group_norm_a_g, group_norm_b_g, w_out, norm_ffn_g, w_router, w_gate, w_up,
              w_down, final_norm_g):
    o_u = 0
    o_v = o_u + A_WIDTH
    o_q = o_v + A_WIDTH
    o_k = o_q + B_WIDTH
    o_vv = o_k + KV_WIDTH
    for l in range(DEPTH):
        h = rms_norm(x, norm_mix_g[l])
        p = jnp.einsum('bsd,dn->bsn', h, w_in[l])
        a_out = chunked_spatial_gating(p[..., o_u:o_v], p[..., o_v:o_q],
                                       gmlp_v_norm_g[l], gmlp_w_s[l], gmlp_b_s[l])
        b_out = grid_attention(p[..., o_q:o_k], p[..., o_k:o_vv], p[..., o_vv:],
                               q_norm_g[l], k_norm_g[l])
        mixed = jnp.concatenate([rms_norm(a_out, group_norm_a_g[l]),
                                 rms_norm(b_out, group_norm_b_g[l])], axis=-1)
        x = x + jnp.einsum('bsm,md->bsd', mixed, w_out[l])
        h2 = rms_norm(x, norm_ffn_g[l])
        x = x + expert_choice_ffn(h2, w_router[l], w_gate[l], w_up[l], w_down[l])
    return rms_norm(x, final_norm_g)
```

```python
import numpy as np
import concourse.bass as bass
import concourse.mybir as mybir
from concourse.bass_utils import run_bass_kernel_spmd

F32 = mybir.dt.float32
BF16 = mybir.dt.bfloat16
U32 = mybir.dt.uint32
I32 = mybir.dt.int32
AF = mybir.ActivationFunctionType
ALU = mybir.AluOpType
AX = mybir.AxisListType

NCORES = 8
D = 1024
S = 2048
NSEQ = 2
T = NSEQ * S
NT = S // 128
INW = 1792
NE = 16
CAP = 256
EPS = 1e-6
GELU_C = 1.5957691216057308

ENG = ("pe", "act", "dve", "pool", "sp")
SAME_ENG_SYNC = True
NDSEM = 8
NO_CC = False
GELU_NATIVE = True
SKIP_WEIGHTS = False
NO_POOL_COMPUTE = False


class Buf:
    __slots__ = ("name", "w", "r", "dead")

    def __init__(self, name, alias=()):
        self.name = name
        self.w = []
        self.r = []
        self.dead = False
        for b in alias:
            self.r += b.w + b.r
            b.dead = True


class Op:
    __slots__ = ("eng", "fn", "deps", "is_dma", "need_inc", "tok", "prev", "idx", "own")

    def __init__(self, eng, fn, is_dma):
        self.own = False
        self.eng = eng
        self.fn = fn
        self.is_dma = is_dma
        self.need_inc = False
        self.tok = None
        self.prev = None
        self.deps = []


class Tl:
    __slots__ = ("ap", "buf")

    def __init__(self, ap, buf):
        self.ap = ap
        self.buf = buf


class Prog:
    def __init__(self, nc):
        self.nc = nc
        self.ops = {e: [] for e in ENG}

    def op(self, eng, fn, r=(), w=(), dma=False, own=False):
        if eng == "pool" and not dma and not own and NO_POOL_COMPUTE:
            eng = "dve"
        o = Op(eng, fn, dma)
        o.own = own
        o.idx = len(self.ops[eng])
        deps = {}
        cand = []
        for b in r:
            assert not b.dead, b.name
            cand += b.w
        for b in w:
            assert not b.dead, b.name
            cand += b.w
            cand += b.r
        for d in cand:
            if d is o:
                continue
            if d.is_dma or d.own:
                deps[id(d)] = d
            else:
                if d.eng == eng and not dma:
                    if eng == "pe" or not SAME_ENG_SYNC:
                        continue
                k = "E" + d.eng
                if k not in deps or deps[k].idx < d.idx:
                    deps[k] = d
        o.deps = list(deps.values())
        for d in o.deps:
            d.need_inc = True
        for b in r:
            b.r.append(o)
        for b in w:
            b.w = [o]
            b.r = []
        self.ops[eng].append(o)
        return o

    def emit(self):
        nc = self.nc
        esem = {e: nc.alloc_semaphore("es_" + e) for e in ENG}
        dsem = {e: [nc.alloc_semaphore("ds_%s%d" % (e, i)) for i in range(NDSEM)] for e in ENG}
        final = {e: {} for e in ENG}
        extra_sems = []
        for e in ENG:
            cnt = 0
            nd = 0
            uses = [0] * NDSEM
            for o in self.ops[e]:
                if o.is_dma:
                    k = nd % NDSEM
                    nd += 1
                    uses[k] += 1
                    o.tok = (dsem[e][k], 16 * uses[k])
                    o.prev = (dsem[e][k], 16 * (uses[k] - 1))
                    final[e][k] = o.tok
                elif o.own:
                    o.tok = (nc.alloc_semaphore("own_%s%d" % (e, o.idx)), 1)
                    extra_sems.append(o.tok[0])
                elif o.need_inc:
                    cnt += 1
                    o.tok = (esem[e], cnt)
        engobj = {"pe": "tensor", "act": "scalar", "dve": "vector", "pool": "gpsimd", "sp": "sync"}

        def emit_engine(e, eng):
            waited = {}

            def wait(tok):
                sem, val = tok
                if val <= 0:
                    return
                if waited.get(sem.num, 0) < val:
                    eng.wait_ge(sem, val)
                    waited[sem.num] = val

            for o in self.ops[e]:
                for d in o.deps:
                    wait(d.tok)
                if o.is_dma:
                    wait(o.prev)
                inst = o.fn(eng)
                if o.is_dma:
                    inst.then_inc(o.tok[0], 16)
                elif o.own:
                    inst.then_inc(o.tok[0])
                    final[e]["own%d" % o.idx] = o.tok
                elif o.need_inc:
                    inst.then_inc(o.tok[0], 1)
            for k, tok in final[e].items():
                wait(tok)

        allsems = list(esem.values()) + [x for e in ENG for x in dsem[e]] + extra_sems
        for sm_ in allsems:
            nc.gpsimd.sem_clear(sm_)
        nc.all_engine_barrier()
        with nc.Block() as blk:
            for e in ENG:
                if not self.ops[e]:
                    continue
                getattr(blk, engobj[e])(lambda eng, e=e: emit_engine(e, eng))
        for sm_ in allsems:
            nc.gpsimd.sem_clear(sm_)
        nc.all_engine_barrier()


class Arena:
    def __init__(self, nc, nbytes):
        self.nbytes = nbytes
        self.t = nc.alloc_sbuf_tensor("arena", [128, nbytes // 4], F32)
        self.views = {F32: self.t, BF16: self.t.bitcast(BF16), U32: self.t.bitcast(U32),
                      I32: self.t.bitcast(I32)}
        self.off = 0
        self.hi = 0

    def alloc(self, name, free_shape, dt, alias=(), at=None):
        esz = 2 if dt == BF16 else 4
        n = 1
        for s_ in free_shape:
            n *= s_
        nb = (n * esz + 31) // 32 * 32
        if at is None:
            at = self.off
            self.off += nb
        assert at % 32 == 0
        self.hi = max(self.hi, at + nb, self.off)
        assert self.hi <= self.nbytes, (name, self.hi, self.nbytes)
        v = self.views[dt][:, at // esz: at // esz + n]
        if len(free_shape) == 2:
            v = v.rearrange("p (a b) -> p a b", a=free_shape[0])
        elif len(free_shape) == 3:
            v = v.rearrange("p (a b c) -> p a b c", a=free_shape[0], b=free_shape[1])
        elif len(free_shape) == 4:
            v = v.rearrange("p (a b c d) -> p a b c d", a=free_shape[0], b=free_shape[1], c=free_shape[2])
        return Tl(v, Buf(name, alias))


def build(dbg=None, stop_after=None):
    nc = bass.Bass("TRN2", target_bir_lowering=False)
    P = Prog(nc)

    def din(name, shape, dt=F32):
        return nc.dram_tensor(name, list(shape), dt, kind="ExternalInput").ap()

    x_d = din("x", [T, D])
    w_in_d = din("w_in", [D, INW])
    g_mix_d = din("g_mix", [128, 8])
    wsT_d = din("wsT", [128, 8 * 128])
    bs_d = din("bs", [128, 8])
    gv_d = din("gv_bc", [128, 512])
    ropeC_d = din("ropeC", [128, NT * 64])
    ropeS_d = din("ropeS", [128, NT * 64])
    gq_d = din("gq_bc", [128, 64])
    gqs_d = din("gqs_bc", [128, 64])
    gk_d = din("gk_bc", [128, 64])
    gks_d = din("gks_bc", [128, 64])
    ident_d = din("ident", [128, 128])
    w_out_d = din("w_out", [D, D])
    g_ab_d = din("g_ab", [128, 8])
    gffn_d = din("gffn_bc", [128, D])
    gfin_d = din("gfin_bc", [128, D])
    wr_d = din("w_router", [D, NE])
    NWM = 1 if SKIP_WEIGHTS else (48 if NO_CC else 6)
    wexp_d = din("w_exp", [NWM * D, D])
    out_d = nc.dram_tensor("out", [T, D], F32, kind="ExternalOutput").ap()
    x1_d = nc.dram_tensor("x1_scr", [T, D], F32).ap()
    h2_d = nc.dram_tensor("h2_scr", [T, D], BF16).ap()
    wsrc_d = nc.dram_tensor("wsrc_bf", [NWM * D, D], BF16).ap()
    wall_d = nc.dram_tensor("wall_bf", [NCORES * 6 * D, D], BF16).ap()
    dbg_d = {}
    if dbg:
        for k, (shp, dt) in dbg.items():
            dbg_d[k] = nc.dram_tensor("dbg_" + k, list(shp), dt, kind="ExternalOutput").ap()

    A = Arena(nc, 206 * 1024)
    ps = nc.alloc_psum_tensor("ps", [128, 4096], F32)
    ps_bf = ps.bitcast(BF16)
    pb = [Buf("psum%d" % i) for i in range(8)]

    def psf(b, n=512, off=0):
        return ps[:, b * 512 + off: b * 512 + off + n]

    def psb(b, n=1024, off=0):
        return ps_bf[:, b * 1024 + off: b * 1024 + off + n]

    dram_x1 = [Buf("x1d%d" % s_) for s_ in range(NSEQ)]
    dram_h2 = [Buf("h2d%d" % s_) for s_ in range(NSEQ)]
    dram_out = Buf("outd")
    dram_dbg = Buf("dbgd")

    def dma(q, out, in_, r=(), w=()):
        eng = {"sp": "sp", "pool": "pool", "act": "act"}[q]
        return P.op(eng, lambda e: e.dma_start(out=out, in_=in_), r=r, w=w, dma=True)

    def mm(out, lhsT, rhs, start, stop, r, w):
        return P.op("pe", lambda e: e.matmul(out, lhsT, rhs, start=start, stop=stop), r=r, w=w)

    def tr(out, in_, ident, r, w):
        return P.op("pe", lambda e: e.transpose(out, in_, ident), r=r, w=w)

    def act(out, in_, func, r, w, scale=None, bias=None, accum=None, eng="act"):
        kw = {}
        if scale is not None:
            kw["scale"] = scale
        if bias is not None:
            kw["bias"] = bias
        if accum is not None:
            kw["accum_out"] = accum
        return P.op("act", lambda e: e.activation(out, in_, func, **kw), r=r, w=w)

    def tt(eng, out, in0, in1, op, r, w):
        return P.op(eng, lambda e: e.tensor_tensor(out, in0, in1, op), r=r, w=w)

    def ts(eng, out, in0, s1, op0, r, w, s2=None, op1=None):
        if op1 is None:
            return P.op(eng, lambda e: e.tensor_scalar(out, in0, s1, None, op0), r=r, w=w)
        return P.op(eng, lambda e: e.tensor_scalar(out, in0, s1, s2, op0, op1), r=r, w=w)

    def cp(eng, out, in_, r, w):
        if eng == "act":
            return P.op("act", lambda e: e.copy(out, in_), r=r, w=w)
        return P.op(eng, lambda e: e.tensor_copy(out, in_), r=r, w=w)

    def red(out, in_, op, r, w):
        return P.op("dve", lambda e: e.tensor_reduce(out, in_, AX.X, op), r=r, w=w)

    def rstd_from_ss(ss, n, nm, cols=1):
        act(ss.ap, ss.ap, AF.Sqrt, r=[ss.buf, eps_t.buf], w=[ss.buf], scale=1.0 / n, bias=eps_t.ap[:, 0:1])
        P.op("dve", lambda e: e.reciprocal(ss.ap, ss.ap), r=[ss.buf], w=[ss.buf])

    eps_t = A.alloc("eps", [1], F32)
    P.op("dve", lambda e: e.memset(eps_t.ap, EPS), w=[eps_t.buf])
    ident = A.alloc("ident", [128], F32)
    dma("sp", ident.ap, ident_d, w=[ident.buf])
    identb = A.alloc("identb", [128], BF16)
    cp("dve", identb.ap, ident.ap, r=[ident.buf], w=[identb.buf])
    gmix = A.alloc("gmix", [8], F32)
    dma("sp", gmix.ap, g_mix_d, w=[gmix.buf])
    gab = A.alloc("gab", [8], F32)
    dma("sp", gab.ap, g_ab_d, w=[gab.buf])
    bs = A.alloc("bs", [8], F32)
    dma("sp", bs.ap, bs_d, w=[bs.buf])
    gv = A.alloc("gv", [512], F32)
    dma("sp", gv.ap, gv_d, w=[gv.buf])
    gffn = A.alloc("gffn", [D], F32)
    dma("sp", gffn.ap, gffn_d, w=[gffn.buf])
    gfin = A.alloc("gfin", [D], F32)
    dma("sp", gfin.ap, gfin_d, w=[gfin.buf])
    wr = A.alloc("wr", [8, NE], F32)
    dma("sp", wr.ap, wr_d.rearrange("(k p) e -> p k e", p=128), w=[wr.buf])
    wsT = A.alloc("wsT", [8, 128], BF16)
    dma("pool", wsT.ap, wsT_d.rearrange("p (h q) -> p h q", h=8), w=[wsT.buf])
    Cq = A.alloc("Cq", [NT, 64], F32)
    Sq = A.alloc("Sq", [NT, 64], F32)
    Ck = A.alloc("Ck", [NT, 64], F32)
    Sk = A.alloc("Sk", [NT, 64], F32)
    g4 = A.alloc("g4", [4, 64], F32)
    for i, gd in enumerate((gq_d, gqs_d, gk_d, gks_d)):
        dma("sp", g4.ap[:, i, :], gd, w=[g4.buf])
    dma("sp", Cq.ap, ropeC_d.rearrange("p (i d) -> p i d", i=NT), w=[Cq.buf])
    dma("sp", Sq.ap, ropeS_d.rearrange("p (i d) -> p i d", i=NT), w=[Sq.buf])
    for i, (dst, src, sc) in enumerate(((Ck, Cq, 1.0), (Sk, Sq, 1.0), (Cq, Cq, 0.125), (Sq, Sq, 0.125))):
        gi = {0: 2, 1: 3, 2: 0, 3: 1}[i]
        gb = g4.ap[:, gi:gi + 1, :].to_broadcast([128, NT, 64])
        P.op("dve", lambda e, dst=dst, src=src, gb=gb, sc=sc: e.scalar_tensor_tensor(
            dst.ap, src.ap, sc, gb, ALU.mult, ALU.mult), r=[src.buf, g4.buf], w=[dst.buf])
    wout = A.alloc("wout", [8, D], BF16)
    if stop_after == "c0":
        return _finish(nc, P, A, out_d, dram_out, dma)
    moe_base = A.off
    win = A.alloc("win", [8, INW], BF16)
    stage = [A.alloc("stage%d" % i, [INW], F32) for i in range(2)]
    for k in range(8):
        st = stage[k % 2]
        dma("sp", st.ap, w_in_d[k * 128:(k + 1) * 128, :], w=[st.buf])
        eng = "dve" if k % 2 == 0 else "pool"
        ts(eng, win.ap[:, k, :], st.ap, gmix.ap[:, k:k + 1], ALU.mult, r=[st.buf, gmix.buf], w=[win.buf])
    for k in range(8):
        st = stage[k % 2]
        dma("sp", st.ap[:, 0:D], w_out_d[k * 128:(k + 1) * 128, :], w=[st.buf])
        eng = "dve" if k % 2 == 0 else "pool"
        ts(eng, wout.ap[:, k, :], st.ap[:, 0:D], gab.ap[:, k:k + 1], ALU.mult, r=[st.buf, gab.buf], w=[wout.buf])

    if stop_after == "c1":
        return _finish(nc, P, A, out_d, dram_out, dma)
    qT = A.alloc("qT", [4, S], BF16)
    kT = A.alloc("kT", [2, S], BF16)
    Vaug = A.alloc("Vaug", [NT, 2, 66], BF16)
    mixTa = A.alloc("mixTa", [4, S], BF16)
    p1tmp_base = A.off
    xt = [A.alloc("xt%d" % i, [D], F32) for i in range(2)]
    junk = A.alloc("junk", [D], BF16)
    xT = A.alloc("xT", [8, 128], BF16)
    uv = A.alloc("uv", [D], F32)
    sq = A.alloc("sq", [D], F32)
    vn = A.alloc("vn", [512], BF16)
    Aa = A.alloc("Aa", [512], F32)
    An = A.alloc("An", [512], BF16)
    qk = A.alloc("qk", [640], F32)
    qk2 = A.alloc("qk2", [640], F32)
    qkb = A.alloc("qkb", [512 + 256], BF16)
    small = A.alloc("small", [32], F32)
    p1_end = A.off

    def sm(a, b):
        return small.ap[:, a:b]

    P.op("pool", lambda e: e.memset(Vaug.ap, 1.0), w=[Vaug.buf])

    def phase1_tile(s_, i):
        tok0 = s_ * S + i * 128
        x = xt[i % 2]
        dma("sp", x.ap, x_d[tok0:tok0 + 128, :], w=[x.buf])
        act(junk.ap, x.ap, AF.Square, r=[x.buf], w=[junk.buf, small.buf], accum=sm(0, 1))
        act(sm(0, 1), sm(0, 1), AF.Sqrt, r=[small.buf, eps_t.buf], w=[small.buf], scale=1.0 / D, bias=eps_t.ap[:, 0:1])
        P.op("dve", lambda e: e.reciprocal(sm(0, 1), sm(0, 1)), r=[small.buf], w=[small.buf])
        rstd = sm(0, 1)
        for k in range(8):
            tr(psf(0 + k // 4, 128, (k % 4) * 128), x.ap[:, k * 128:(k + 1) * 128], ident.ap,
               r=[x.buf, ident.buf], w=[pb[k // 4]])
        cp("dve", xT.ap[:, 0:4, :], psf(0).rearrange("p (a b) -> p a b", a=4), r=[pb[0]], w=[xT.buf])
        cp("act", xT.ap[:, 4:8, :], psf(1).rearrange("p (a b) -> p a b", a=4), r=[pb[1]], w=[xT.buf])
        for k in range(8):
            for n in range(4):
                n0 = n * 512
                nn = min(512, INW - n0)
                mm(psf(2 + n, nn), xT.ap[:, k, :], win.ap[:, k, n0:n0 + nn], start=(k == 0), stop=(k == 7),
                   r=[xT.buf, win.buf], w=[pb[2 + n]])
        if stop_after == "tA":
            return
        pu = ps[:, 2 * 512: 2 * 512 + 1024]
        if GELU_NATIVE:
            for b_ in range(2):
                P.op("act", lambda e, b_=b_: e.activation(uv.ap[:, b_ * 512:(b_ + 1) * 512], psf(2 + b_), AF.Gelu_apprx_tanh, scale=rstd),
                     r=[pb[2 + b_], small.buf], w=[uv.buf])
        else:
            for b_ in range(2):
                P.op("act", lambda e, b_=b_: e.activation(uv.ap[:, b_ * 512:(b_ + 1) * 512], psf(2 + b_), AF.Identity, scale=rstd),
                     r=[pb[2 + b_], small.buf], w=[uv.buf])
            act(sq.ap, uv.ap, AF.Square, r=[uv.buf], w=[sq.buf])
            ts("dve", sq.ap, sq.ap, 0.044715, ALU.mult, r=[sq.buf], w=[sq.buf], s2=1.0, op1=ALU.add)
            tt("dve", sq.ap, sq.ap, uv.ap, ALU.mult, r=[sq.buf, uv.buf], w=[sq.buf])
            act(sq.ap, sq.ap, AF.Sigmoid, r=[sq.buf], w=[sq.buf], scale=GELU_C)
            tt("dve", uv.ap, uv.ap, sq.ap, ALU.mult, r=[sq.buf, uv.buf], w=[uv.buf])
        if stop_after == "tB":
            return
        pq = ps[:, 4 * 512: 4 * 512 + 640]
        ts("dve", qk.ap[:, 0:512], psf(4), rstd, ALU.mult, r=[pb[4], small.buf], w=[qk.buf])
        ts("dve", qk.ap[:, 512:640], psf(5, 128), rstd, ALU.mult, r=[pb[5], small.buf], w=[qk.buf])
        P.op("act", lambda e: e.activation(Vaug.ap[:, i, :, 0:64], psf(5, 128, 128).rearrange("p (a b) -> p a b", a=2),
                                           AF.Identity, scale=rstd), r=[pb[5], small.buf], w=[Vaug.buf])
        v = uv.ap[:, 512:1024]
        tt("pool", sq.ap[:, 0:512], v, v, ALU.mult, r=[uv.buf], w=[sq.buf])
        red(sm(8, 16), sq.ap[:, 0:512].rearrange("p (h d) -> p h d", h=8), ALU.add, r=[sq.buf], w=[small.buf])
        act(sm(8, 16), sm(8, 16), AF.Sqrt, r=[small.buf, eps_t.buf], w=[small.buf], scale=1.0 / 64, bias=eps_t.ap[:, 0:1])
        P.op("dve", lambda e: e.reciprocal(sm(8, 16), sm(8, 16)), r=[small.buf], w=[small.buf])
        tt("dve", sq.ap[:, 0:512].rearrange("p (h d) -> p h d", h=8), v.rearrange("p (h d) -> p h d", h=8),
           sm(8, 16).unsqueeze(2).to_broadcast([128, 8, 64]), ALU.mult, r=[uv.buf, small.buf], w=[sq.buf])
        tt("pool", vn.ap, sq.ap[:, 0:512], gv.ap, ALU.mult, r=[sq.buf, gv.buf], w=[vn.buf])
        for h in range(8):
            mm(psf(6, 64, h * 64), wsT.ap[:, h, :], vn.ap[:, h * 64:(h + 1) * 64], start=True, stop=True,
               r=[wsT.buf, vn.buf], w=[pb[6]])
        tt("dve", Aa.ap.rearrange("p (h d) -> p h d", h=8), psf(6).rearrange("p (h d) -> p h d", h=8),
           bs.ap.unsqueeze(2).to_broadcast([128, 8, 64]), ALU.add, r=[pb[6], bs.buf], w=[Aa.buf])
        tt("dve", Aa.ap, Aa.ap, uv.ap[:, 0:512], ALU.mult, r=[Aa.buf, uv.buf], w=[Aa.buf])
        act(junk.ap[:, 0:512], Aa.ap, AF.Square, r=[Aa.buf], w=[junk.buf, small.buf], accum=sm(1, 2))
        act(sm(1, 2), sm(1, 2), AF.Sqrt, r=[small.buf, eps_t.buf], w=[small.buf], scale=1.0 / 512, bias=eps_t.ap[:, 0:1])
        P.op("dve", lambda e: e.reciprocal(sm(1, 2), sm(1, 2)), r=[small.buf], w=[small.buf])
        P.op("act", lambda e: e.activation(An.ap, Aa.ap, AF.Identity, scale=sm(1, 2)), r=[Aa.buf, small.buf], w=[An.buf])
        for c in range(4):
            tr(psb(7, 128, c * 128), An.ap[:, c * 128:(c + 1) * 128], identb.ap, r=[An.buf, identb.buf], w=[pb[7]])
        cp("dve", mixTa.ap[:, :, i * 128:(i + 1) * 128], psb(7, 512).rearrange("p (a b) -> p a b", a=4),
           r=[pb[7]], w=[mixTa.buf])
        if stop_after == "tC":
            return
        tt("pool", qk2.ap, qk.ap, qk.ap, ALU.mult, r=[qk.buf], w=[qk2.buf])
        red(sm(16, 26), qk2.ap.rearrange("p (h d) -> p h d", h=10), ALU.add, r=[qk2.buf], w=[small.buf])
        act(sm(16, 26), sm(16, 26), AF.Sqrt, r=[small.buf, eps_t.buf], w=[small.buf], scale=1.0 / 64, bias=eps_t.ap[:, 0:1])
        P.op("dve", lambda e: e.reciprocal(sm(16, 26), sm(16, 26)), r=[small.buf], w=[small.buf])
        tt("dve", qk.ap.rearrange("p (h d) -> p h d", h=10), qk.ap.rearrange("p (h d) -> p h d", h=10),
           sm(16, 26).unsqueeze(2).to_broadcast([128, 10, 64]), ALU.mult, r=[qk.buf, small.buf], w=[qk.buf])
        if stop_after == "tD":
            return
        for (lo, nh, Ct, St) in ((0, 8, Cq, Sq), (512, 2, Ck, Sk)):
            xv = qk.ap[:, lo:lo + nh * 64]
            x5 = xv.rearrange("p (h a b d) -> p h a b d", h=nh, a=2, b=2)
            o2 = qk2.ap[:, lo:lo + nh * 64]
            o5 = o2.rearrange("p (h a b d) -> p h a b d", h=nh, a=2, b=2)
            S5 = St.ap[:, i, :].rearrange("p (a b d) -> p a b d", a=2, b=2)
            for a_ in range(2):
                for b_ in range(2):
                    tt("pool", o5[:, :, a_, b_, :], x5[:, :, a_, 1 - b_, :],
                       S5[:, a_, b_, :].unsqueeze(1).to_broadcast([128, nh, 16]), ALU.mult,
                       r=[qk.buf, St.buf], w=[qk2.buf])
            x3 = xv.rearrange("p (h d) -> p h d", h=nh)
            tt("dve", x3, x3, Ct.ap[:, i:i + 1, :].to_broadcast([128, nh, 64]), ALU.mult,
               r=[qk.buf, Ct.buf], w=[qk.buf])
        tt("dve", qkb.ap[:, 0:512], qk.ap[:, 0:512], qk2.ap[:, 0:512], ALU.add, r=[qk.buf, qk2.buf], w=[qkb.buf])
        kd = qkb.ap[:, 512:768].rearrange("p (kv r d) -> p kv r d", kv=2, r=2)
        for r_ in range(2):
            tt("pool", kd[:, :, r_, :], qk.ap[:, 512:640].rearrange("p (kv d) -> p kv d", kv=2),
               qk2.ap[:, 512:640].rearrange("p (kv d) -> p kv d", kv=2), ALU.add,
               r=[qk.buf, qk2.buf], w=[qkb.buf])
        if stop_after == "tE":
            return
        for c in range(4):
            tr(psb(0, 128, c * 128), qkb.ap[:, c * 128:(c + 1) * 128], identb.ap, r=[qkb.buf, identb.buf], w=[pb[0]])
        for c in range(2):
            tr(psb(0, 128, 512 + c * 128), qkb.ap[:, 512 + c * 128:512 + (c + 1) * 128], identb.ap,
               r=[qkb.buf, identb.buf], w=[pb[0]])
        cp("dve", qT.ap[:, :, i * 128:(i + 1) * 128], psb(0, 512).rearrange("p (a b) -> p a b", a=4),
           r=[pb[0]], w=[qT.buf])
        cp("dve", kT.ap[:, :, i * 128:(i + 1) * 128], psb(0, 256, 512).rearrange("p (a b) -> p a b", a=2),
           r=[pb[0]], w=[kT.buf])
        if dbg and s_ == 0 and i == 1:
            if "uv" in dbg_d:
                dma("sp", dbg_d["uv"], uv.ap, r=[uv.buf], w=[dram_dbg])
            if "Aa" in dbg_d:
                dma("sp", dbg_d["Aa"], Aa.ap, r=[Aa.buf], w=[dram_dbg])
            if "qkb" in dbg_d:
                dma("sp", dbg_d["qkb"], qkb.ap, r=[qkb.buf], w=[dram_dbg])
            if "vn" in dbg_d:
                dma("sp", dbg_d["vn"], vn.ap, r=[vn.buf], w=[dram_dbg])

    wall_buf = Buf("wall")
    wsrc_buf = Buf("wsrc")
    ws_at = A.off
    wstage = [A.alloc("wstage%d" % i, [4, D], BF16) for i in range(2)]
    for j in range(0 if SKIP_WEIGHTS else NWM):
        for hlf in range(2):
            rows = slice(j * D + hlf * 512, j * D + (hlf + 1) * 512)
            dma("pool", wstage[hlf].ap, wexp_d[rows, :].rearrange("(k p) n -> p k n", p=128), w=[wstage[hlf].buf])
            dma("sp", wsrc_d[rows, :].rearrange("(k p) n -> p k n", p=128), wstage[hlf].ap,
                r=[wstage[hlf].buf], w=[wsrc_buf])
    if not NO_CC and not SKIP_WEIGHTS:
        P.op("pool", lambda e: e.collective_compute("AllGather", ALU.bypass, replica_groups=[list(range(NCORES))],
                                                    ins=[wsrc_d.opt()], outs=[wall_d.opt()]),
             r=[wsrc_buf], w=[wall_buf], own=True)
    PT = [A.alloc("PT%d" % i, [1024], BF16, alias=[wstage[0].buf], at=ws_at + i * 2048) for i in range(4)]
    Bo = A.alloc("Bo", [4, 512], F32, alias=[wstage[1].buf], at=ws_at + 8192)
    OT = [A.alloc("OT%d" % i, [512], F32) for i in range(2)]
    aff64 = A.alloc("aff64", [64], F32)
    affT = A.alloc("affT", [S], F32)
    tkv = A.alloc("tkv", [CAP], F32)
    tki = A.alloc("tki", [CAP], U32)
    tkf = A.alloc("tkf", [CAP], F32)
    idxT = A.alloc("idxT", [NSEQ, 2, NE], I32)
    gT = A.alloc("gT", [NSEQ, 2, NE], F32)
    P.op("pool", lambda e: e.memset(aff64.ap, 0.0), w=[aff64.buf])

    def wmat_src(e, m):
        if NO_CC:
            base = (e * 3 + m) * D
            return wsrc_d[base:base + D, :], wsrc_buf
        base = ((e // 2) * 6 + (e % 2) * 3 + m) * D
        return wall_d[base:base + D, :], wall_buf

    def topk_rounds(s_):
        rows = slice(s_ * 32, s_ * 32 + NE)
        R = affT.ap[rows, :]
        for r_ in range(CAP // 8):
            v8 = tkv.ap[rows, r_ * 8:(r_ + 1) * 8]
            i8 = tki.ap[rows, r_ * 8:(r_ + 1) * 8]
            P.op("dve", lambda e, v8=v8: e.max(v8, R), r=[affT.buf], w=[tkv.buf])
            P.op("dve", lambda e, v8=v8, i8=i8: e.max_index(i8, v8, R), r=[affT.buf, tkv.buf], w=[tki.buf])
            P.op("dve", lambda e, v8=v8: e.match_replace(R, v8, R, -1.0), r=[tkv.buf, affT.buf], w=[affT.buf])
            yield
        ts("dve", tkf.ap[rows, :], tki.ap[rows, :], float(s_ * S), ALU.add, r=[tki.buf], w=[tkf.buf])
        idn = ident.ap[rows, s_ * 32: s_ * 32 + NE]
        for hf in range(2):
            tr(psf(6, NE, hf * 32), tkf.ap[rows, hf * 128:(hf + 1) * 128], idn, r=[tkf.buf, ident.buf], w=[pb[6]])
            tr(psf(6, NE, 64 + hf * 32), tkv.ap[rows, hf * 128:(hf + 1) * 128], idn, r=[tkv.buf, ident.buf], w=[pb[6]])
        for hf in range(2):
            cp("dve", idxT.ap[:, s_, hf, :], psf(6, NE, hf * 32), r=[pb[6]], w=[idxT.buf])
            cp("dve", gT.ap[:, s_, hf, :], psf(6, NE, 64 + hf * 32), r=[pb[6]], w=[gT.buf])
        yield

    def drain(gen):
        if gen is not None:
            for _ in gen:
                pass

    def step(gen):
        if gen is not None:
            try:
                next(gen)
            except StopIteration:
                pass

    def phase3_tile(s_, i, t):
        tok0 = s_ * S + i * 128
        Bt = Bo.ap[:, t, :]
        act(junk.ap[:, 0:512], Bt, AF.Square, r=[Bo.buf], w=[junk.buf, small.buf], accum=sm(2, 3))
        act(sm(2, 3), sm(2, 3), AF.Sqrt, r=[small.buf, eps_t.buf], w=[small.buf], scale=1.0 / 512, bias=eps_t.ap[:, 0:1])
        P.op("dve", lambda e: e.reciprocal(sm(2, 3), sm(2, 3)), r=[small.buf], w=[small.buf])
        P.op("act", lambda e: e.activation(An.ap, Bt, AF.Identity, scale=sm(2, 3)), r=[Bo.buf, small.buf], w=[An.buf])
        for c in range(4):
            tr(psb(6, 128, c * 128), An.ap[:, c * 128:(c + 1) * 128], identb.ap, r=[An.buf, identb.buf], w=[pb[6]])
        mTb = vn.ap.rearrange("p (a b) -> p a b", a=4)
        cp("dve", mTb, psb(6, 512).rearrange("p (a b) -> p a b", a=4), r=[pb[6]], w=[vn.buf])
        for k in range(8):
            if k < 4:
                lh, lb = mixTa.ap[:, k, i * 128:(i + 1) * 128], mixTa.buf
            else:
                lh, lb = mTb[:, k - 4, :], vn.buf
            for n in range(2):
                mm(psf(n), lh, wout.ap[:, k, n * 512:(n + 1) * 512], start=(k == 0), stop=(k == 7),
                   r=[lb, wout.buf], w=[pb[n]])
        xa, x1 = xt[0], xt[1]
        dma("sp", xa.ap, x_d[tok0:tok0 + 128, :], w=[xa.buf])
        for n in range(2):
            tt("dve", x1.ap[:, n * 512:(n + 1) * 512], xa.ap[:, n * 512:(n + 1) * 512], psf(n), ALU.add,
               r=[xa.buf, pb[n]], w=[x1.buf])
        dma("sp", x1_d[tok0:tok0 + 128, :], x1.ap, r=[x1.buf], w=[dram_x1[s_]])
        act(junk.ap, x1.ap, AF.Square, r=[x1.buf], w=[junk.buf, small.buf], accum=sm(3, 4))
        act(sm(3, 4), sm(3, 4), AF.Sqrt, r=[small.buf, eps_t.buf], w=[small.buf], scale=1.0 / D, bias=eps_t.ap[:, 0:1])
        P.op("dve", lambda e: e.reciprocal(sm(3, 4), sm(3, 4)), r=[small.buf], w=[small.buf])
        P.op("dve", lambda e: e.scalar_tensor_tensor(uv.ap, x1.ap, sm(3, 4), gffn.ap, ALU.mult, ALU.mult),
             r=[x1.buf, small.buf, gffn.buf], w=[uv.buf])
        cp("act", junk.ap, uv.ap, r=[uv.buf], w=[junk.buf])
        dma("sp", h2_d[tok0:tok0 + 128, :], junk.ap, r=[junk.buf], w=[dram_h2[s_]])
        for k in range(8):
            tr(psf(2 + k // 4, 128, (k % 4) * 128), uv.ap[:, k * 128:(k + 1) * 128], ident.ap,
               r=[uv.buf, ident.buf], w=[pb[2 + k // 4]])
        h2T = sq.ap.rearrange("p (a b) -> p a b", a=8)
        cp("dve", h2T[:, 0:4, :], psf(2).rearrange("p (a b) -> p a b", a=4), r=[pb[2]], w=[sq.buf])
        cp("act", h2T[:, 4:8, :], psf(3).rearrange("p (a b) -> p a b", a=4), r=[pb[3]], w=[sq.buf])
        lg = psf(6, NE, 256)
        for k in range(8):
            mm(lg, h2T[:, k, :], wr.ap[:, k, :], start=(k == 0), stop=(k == 7), r=[sq.buf, wr.buf], w=[pb[6]])
        P.op("dve", lambda e: e.tensor_reduce(sm(4, 5), lg, AX.X, ALU.max), r=[pb[6]], w=[small.buf])
        ts("dve", sm(4, 5), sm(4, 5), -1.0, ALU.mult, r=[small.buf], w=[small.buf])
        afs = aff64.ap[:, s_ * 32: s_ * 32 + NE]
        P.op("act", lambda e: e.activation(afs, lg, AF.Exp, bias=sm(4, 5), accum_out=sm(5, 6)),
             r=[pb[6], small.buf], w=[aff64.buf, small.buf])
        P.op("dve", lambda e: e.reciprocal(sm(5, 6), sm(5, 6)), r=[small.buf], w=[small.buf])
        ts("dve", afs, afs, sm(5, 6), ALU.mult, r=[aff64.buf, small.buf], w=[aff64.buf])
        tr(psf(6, 128, 384)[0:64, :], aff64.ap, ident.ap, r=[aff64.buf, ident.buf], w=[pb[6]])
        rows = slice(s_ * 32, s_ * 32 + NE)
        cp("dve", affT.ap[rows, i * 128:(i + 1) * 128], psf(6, 128, 384)[rows, :], r=[pb[6]], w=[affT.buf])

    def attn_head(s_, qb, h):
        kv, half, c = h // 4, h % 2, h // 2
        prt = slice(half * 64, half * 64 + 64)
        qTs = qT.ap[prt, c, qb * 512:(qb + 1) * 512]
        ob = 4 + (h % 2)

        def qk(g):
            for j in range(2):
                kt = 2 * g + j
                bank = (g % 2) * 2 + j
                mm(psf(bank), kT.ap[prt, kv, kt * 128:(kt + 1) * 128], qTs, start=True, stop=True,
                   r=[kT.buf, qT.buf], w=[pb[bank]])

        def ex(g):
            b0 = (g % 2) * 2
            pt = PT[g % 4]
            for j in range(2):
                P.op("act", lambda e, j=j: e.activation(pt.ap[:, j * 512:(j + 1) * 512], psf(b0 + j), AF.Exp),
                     r=[pb[b0 + j]], w=[pt.buf])

        def pv(g):
            pt = PT[g % 4]
            for j in range(2):
                kt = 2 * g + j
                mm(psf(ob)[0:65, :], Vaug.ap[:, kt, kv, 0:65], pt.ap[:, j * 512:(j + 1) * 512],
                   start=(kt == 0), stop=(kt == NT - 1), r=[Vaug.buf, pt.buf], w=[pb[ob]])

        qk(0)
        for g in range(8):
            if g + 1 < 8:
                qk(g + 1)
            ex(g)
            pv(g)
        ot = OT[h % 2]
        cp("dve", ot.ap[0:65, :], psf(ob)[0:65, :], r=[pb[ob]], w=[ot.buf])
        for t in range(4):
            tr(psf(6, 65, t * 128), ot.ap[0:65, t * 128:(t + 1) * 128], ident.ap[0:65, 0:65],
               r=[ot.buf, ident.buf], w=[pb[6]])
        for t in range(4):
            P.op("dve", lambda e, t=t: e.reciprocal(sm(24 + t, 25 + t), psf(6, 1, t * 128 + 64)), r=[pb[6]], w=[small.buf])
            ts("dve", Bo.ap[:, t, h * 64:(h + 1) * 64], psf(6, 64, t * 128), sm(24 + t, 25 + t), ALU.mult,
               r=[pb[6], small.buf], w=[Bo.buf])

    pending_topk = None
    for s_ in range(NSEQ):
        for i in range(NT):
            phase1_tile(s_, i)
            if stop_after in ("t1", "tA", "tB", "tC", "tD", "tE") and i == 1:
                break
        if stop_after in ("p1", "t1", "tA", "tB", "tC", "tD", "tE"):
            break
        for qb in range(4):
            for h in range(8):
                attn_head(s_, qb, h)
                step(pending_topk)
            for t in range(4):
                phase3_tile(s_, qb * 4 + t, t)
            if stop_after == "q1":
                break
        if stop_after == "q1":
            break
        drain(pending_topk)
        pending_topk = topk_rounds(s_)
        if stop_after == "s1":
            break
    drain(pending_topk)
    if dbg:
        for nm, tl in (("qT", qT), ("kT", kT), ("Vaug", Vaug), ("mixTa", mixTa), ("Bo", Bo), ("affT", affT),
                       ("tkv", tkv), ("tki", tki), ("idxT", idxT), ("gT", gT)):
            if stop_after == "q1" and nm in ("affT", "tkv", "tki", "idxT", "gT"):
                continue
            if nm in dbg_d:
                if nm in ("affT", "tkv", "tki"):
                    dma("sp", dbg_d[nm][0:16, :], tl.ap[0:16, :], r=[tl.buf], w=[dram_dbg])
                elif nm in ("idxT", "gT"):
                    dma("sp", dbg_d[nm][:, 0:32], tl.ap[:, 0, :, :], r=[tl.buf], w=[dram_dbg])
                else:
                    dma("sp", dbg_d[nm], tl.ap, r=[tl.buf], w=[dram_dbg])
        if "x1" in dbg_d:
            nrow = 512 if stop_after == "q1" else S
            dma("sp", dbg_d["x1"][0:nrow, :], x1_d[0:nrow, :], r=[dram_x1[0]], w=[dram_dbg])
    if stop_after is not None:
        return _finish(nc, P, A, out_d, dram_out, dma)

    old = [win.buf, stage[0].buf, stage[1].buf, qT.buf, kT.buf, Vaug.buf, mixTa.buf]
    mo = moe_base
    wslot = []
    for i in range(3):
        wslot.append(A.alloc("wslot%d" % i, [8, D], BF16, alias=old if i == 0 else (), at=mo))
        mo += 16384
    if True:
        for wsl in wslot[1:]:
            wsl.buf.r += wslot[0].buf.r
    def malloc(name, shape, dt):
        nonlocal mo
        tl = A.alloc(name, shape, dt, at=mo)
        n = 1
        for q_ in shape:
            n *= q_
        mo += (n * (2 if dt == BF16 else 4) + 31) // 32 * 32
        tl.buf.r += wslot[0].buf.r
        return tl
    pre_moe_ops = list(wslot[0].buf.r)
    xs = malloc("xs", [4, D], BF16)
    xsT = malloc("xsT", [8, 512], BF16)
    hT = malloc("hT", [8, 512], BF16)
    sil = [malloc("sil%d" % i, [512], F32) for i in range(2)]
    yb = [malloc("yb%d" % i, [D], F32) for i in range(2)]
    assert mo <= p1tmp_base, (mo, p1tmp_base)

    nw = 0
    def load_w(e, m):
        nonlocal nw
        sl = wslot[nw % 3]
        nw += 1
        src, sbuf_ = wmat_src(e, m)
        for hlf in range(2):
            dma("sp", sl.ap[:, hlf * 4:(hlf + 1) * 4, :],
                src[hlf * 512:(hlf + 1) * 512, :].rearrange("(k p) n -> p k n", p=128), r=[sbuf_], w=[sl.buf])
        return sl

    for e in range(NE):
        for ct in range(4):
            s_, hf = ct // 2, ct % 2
            P.op("pool", lambda en, ct=ct, s_=s_, hf=hf, e=e: en.indirect_dma_start(
                out=xs.ap[:, ct, :], out_offset=None, in_=h2_d,
                in_offset=bass.IndirectOffsetOnAxis(ap=idxT.ap[:, s_, hf, e:e + 1], axis=0)),
                r=[idxT.buf, dram_h2[0], dram_h2[1]], w=[xs.buf], dma=True)
        for ct in range(4):
            for k in range(8):
                tr(psb(ct, 128, k * 128), xs.ap[:, ct, k * 128:(k + 1) * 128], identb.ap,
                   r=[xs.buf, identb.buf], w=[pb[ct]])
            cp("dve", xsT.ap[:, :, ct * 128:(ct + 1) * 128],
               psb(ct, 1024).rearrange("p (k c) -> p k c", k=8), r=[pb[ct]], w=[xsT.buf])
        Wg = load_w(e, 0)
        Wu = load_w(e, 1)
        for ft in range(8):
            ba = 4 + (ft % 2) * 2
            bb = ba + 1
            for (bk, W_) in ((ba, Wg), (bb, Wu)):
                for k in range(8):
                    mm(psf(bk), W_.ap[:, k, ft * 128:(ft + 1) * 128], xsT.ap[:, k, :], start=(k == 0), stop=(k == 7),
                       r=[W_.buf, xsT.buf], w=[pb[bk]])
            sl_ = sil[ft % 2]
            act(sl_.ap, psf(ba), AF.Silu, r=[pb[ba]], w=[sl_.buf])
            tt("dve", hT.ap[:, ft, :], sl_.ap, psf(bb), ALU.mult, r=[sl_.buf, pb[bb]], w=[hT.buf])
        Wd = load_w(e, 2)
        for ct in range(4):
            s_, hf = ct // 2, ct % 2
            y = yb[ct % 2]
            gsc = gT.ap[:, s_, hf, e:e + 1]
            for dh in range(2):
                bk = (ct * 2 + dh) % 4
                for k in range(8):
                    mm(psf(bk), hT.ap[:, k, ct * 128:(ct + 1) * 128], Wd.ap[:, k, dh * 512:(dh + 1) * 512],
                       start=(k == 0), stop=(k == 7), r=[hT.buf, Wd.buf], w=[pb[bk]])
                if dh == 0:
                    P.op("act", lambda en, y=y, bk=bk, gsc=gsc: en.activation(y.ap[:, 0:512], psf(bk), AF.Identity, scale=gsc),
                         r=[pb[bk], gT.buf], w=[y.buf])
                else:
                    ts("dve", y.ap[:, 512:1024], psf(bk), gsc, ALU.mult, r=[pb[bk], gT.buf], w=[y.buf])
            P.op("pool", lambda en, y=y, s_=s_, hf=hf, e=e: en.indirect_dma_start(
                out=x1_d, out_offset=bass.IndirectOffsetOnAxis(ap=idxT.ap[:, s_, hf, e:e + 1], axis=0),
                in_=y.ap, in_offset=None, compute_op=ALU.add),
                r=[y.buf, idxT.buf], w=[dram_x1[s_]], dma=True)

    for s_ in range(NSEQ):
        for i in range(NT):
            tok0 = s_ * S + i * 128
            xa = xt[i % 2]
            dma("sp", xa.ap, x1_d[tok0:tok0 + 128, :], r=[dram_x1[s_]], w=[xa.buf])
            act(junk.ap, xa.ap, AF.Square, r=[xa.buf], w=[junk.buf, small.buf], accum=sm(6, 7))
            act(sm(6, 7), sm(6, 7), AF.Sqrt, r=[small.buf, eps_t.buf], w=[small.buf], scale=1.0 / D, bias=eps_t.ap[:, 0:1])
            P.op("dve", lambda e: e.reciprocal(sm(6, 7), sm(6, 7)), r=[small.buf], w=[small.buf])
            P.op("dve", lambda e, xa=xa: e.scalar_tensor_tensor(uv.ap, xa.ap, sm(6, 7), gfin.ap, ALU.mult, ALU.mult),
                 r=[xa.buf, small.buf, gfin.buf], w=[uv.buf])
            dma("sp", out_d[tok0:tok0 + 128, :], uv.ap, r=[uv.buf], w=[dram_out])
    P.emit()
    return nc


def _finish(nc, P, A, out_d, dram_out, dma):
    z = A.alloc("z", [D], F32)
    P.op("dve", lambda e: e.memset(z.ap, 0.0), w=[z.buf])
    dma("sp", out_d[0:128, :], z.ap, r=[z.buf], w=[dram_out])
    P.emit()
    return nc


def _rope_tables():
    half = 16
    inv_freq = (np.float32(10000.0) ** (-np.arange(half, dtype=np.float32) / np.float32(half))).astype(np.float32)
    C = np.zeros((128, NT, 64), np.float32)
    Sg = np.zeros((128, NT, 64), np.float32)
    p = np.arange(128)
    for i in range(NT):
        t = 128 * i + p
        row = (t // 64).astype(np.float32)
        col = (t % 64).astype(np.float32)
        ar = (row[:, None] * inv_freq[None, :]).astype(np.float32)
        ac = (col[:, None] * inv_freq[None, :]).astype(np.float32)
        C[:, i, 0:16] = np.cos(ar); C[:, i, 16:32] = np.cos(ar)
        C[:, i, 32:48] = np.cos(ac); C[:, i, 48:64] = np.cos(ac)
        Sg[:, i, 0:16] = -np.sin(ar); Sg[:, i, 16:32] = np.sin(ar)
        Sg[:, i, 32:48] = -np.sin(ac); Sg[:, i, 48:64] = np.sin(ac)
    return C.reshape(128, NT * 64), Sg.reshape(128, NT * 64)


def _swap(g):
    g = np.asarray(g, np.float32).reshape(2, 2, 16)
    return g[:, ::-1, :].reshape(64)


def make_in_maps(inputs):
    f = lambda a: np.ascontiguousarray(np.asarray(a, dtype=np.float32))
    x = f(inputs["x"])
    C, Sg = _rope_tables()
    gq = f(inputs["q_norm_g"]).reshape(64)
    gk = f(inputs["k_norm_g"]).reshape(64)
    shared = {
        "w_in": f(inputs["w_in"])[0],
        "g_mix": f(f(inputs["norm_mix_g"]).reshape(8, 128).T),
        "wsT": f(f(inputs["gmlp_w_s"])[0].transpose(2, 0, 1).reshape(128, 8 * 128)),
        "bs": f(f(inputs["gmlp_b_s"])[0].T),
        "gv_bc": f(np.tile(f(inputs["gmlp_v_norm_g"]).reshape(1, 512), (128, 1))),
        "ropeC": C, "ropeS": Sg,
        "gq_bc": f(np.tile(gq[None], (128, 1))), "gqs_bc": f(np.tile(_swap(gq)[None], (128, 1))),
        "gk_bc": f(np.tile(gk[None], (128, 1))), "gks_bc": f(np.tile(_swap(gk)[None], (128, 1))),
        "ident": np.eye(128, dtype=np.float32),
        "w_out": f(inputs["w_out"])[0],
        "g_ab": f(np.concatenate([f(inputs["group_norm_a_g"]).reshape(-1), f(inputs["group_norm_b_g"]).reshape(-1)]).reshape(8, 128).T),
        "gffn_bc": f(np.tile(f(inputs["norm_ffn_g"]).reshape(1, D), (128, 1))),
        "gfin_bc": f(np.tile(f(inputs["final_norm_g"]).reshape(1, D), (128, 1))),
        "w_router": f(inputs["w_router"])[0],
    }
    wg = f(inputs["w_gate"])[0]; wu = f(inputs["w_up"])[0]; wd = f(inputs["w_down"])[0]
    maps = []
    for c in range(NCORES):
        m = dict(shared)
        m["x"] = np.ascontiguousarray(x[c * NSEQ:(c + 1) * NSEQ].reshape(T, D))
        el = range(NE) if NO_CC else (2 * c, 2 * c + 1)
        if SKIP_WEIGHTS:
            m["w_exp"] = np.zeros((D, D), np.float32)
            maps.append(m)
            continue
        m["w_exp"] = np.ascontiguousarray(np.concatenate([w[e] for e in el for w in (wg, wu, wd)], axis=0))
        maps.append(m)
    return maps


def kernel(**inputs):
    nc = build()
    maps = make_in_maps(inputs)
    res = run_bass_kernel_spmd(nc, maps, core_ids=list(range(NCORES)))
    out = np.stack([np.asarray(r["out"]).reshape(NSEQ, S, D) for r in res.results], axis=0)
    return out.reshape(NCORES * NSEQ, S, D).astype(np.float32)
```

```python
import numpy as np
import concourse.bass as bass
import concourse.mybir as mybir
from concourse.bass_utils import run_bass_kernel_spmd

F32 = mybir.dt.float32
BF16 = mybir.dt.bfloat16
U32 = mybir.dt.uint32
I32 = mybir.dt.int32
AF = mybir.ActivationFunctionType
ALU = mybir.AluOpType
AX = mybir.AxisListType

NCORES = 8
D = 1024
S = 2048
NSEQ = 2
T = NSEQ * S
NT = S // 128
INW = 1792
NE = 16
CAP = 256
EPS = 1e-6
GELU_C = 1.5957691216057308

ENG = ("pe", "act", "dve", "pool", "sp")
SAME_ENG_SYNC = True
NDSEM = 8
NO_CC = False
GELU_NATIVE = True
SKIP_WEIGHTS = False
NO_POOL_COMPUTE = False


class Buf:
    __slots__ = ("name", "w", "r", "dead")

    def __init__(self, name, alias=()):
        self.name = name
        self.w = []
        self.r = []
        self.dead = False
        for b in alias:
            self.r += b.w + b.r
            b.dead = True


class Op:
    __slots__ = ("eng", "fn", "deps", "is_dma", "need_inc", "tok", "prev", "idx", "own")

    def __init__(self, eng, fn, is_dma):
        self.own = False
        self.eng = eng
        self.fn = fn
        self.is_dma = is_dma
        self.need_inc = False
        self.tok = None
        self.prev = None
        self.deps = []


class Tl:
    __slots__ = ("ap", "buf")

    def __init__(self, ap, buf):
        self.ap = ap
        self.buf = buf


class Prog:
    def __init__(self, nc):
        self.nc = nc
        self.ops = {e: [] for e in ENG}

    def op(self, eng, fn, r=(), w=(), dma=False, own=False):
        if eng == "pool" and not dma and not own and NO_POOL_COMPUTE:
            eng = "dve"
        o = Op(eng, fn, dma)
        o.own = own
        o.idx = len(self.ops[eng])
        deps = {}
        cand = []
        for b in r:
            assert not b.dead, b.name
            cand += b.w
        for b in w:
            assert not b.dead, b.name
            cand += b.w
            cand += b.r
        for d in cand:
            if d is o:
                continue
            if d.is_dma or d.own:
                deps[id(d)] = d
            else:
                if d.eng == eng and not dma:
                    if eng == "pe" or not SAME_ENG_SYNC:
                        continue
                k = "E" + d.eng
                if k not in deps or deps[k].idx < d.idx:
                    deps[k] = d
        o.deps = list(deps.values())
        for d in o.deps:
            d.need_inc = True
        for b in r:
            b.r.append(o)
        for b in w:
            b.w = [o]
            b.r = []
        self.ops[eng].append(o)
        return o

    def emit(self):
        nc = self.nc
        esem = {e: nc.alloc_semaphore("es_" + e) for e in ENG}
        dsem = {e: [nc.alloc_semaphore("ds_%s%d" % (e, i)) for i in range(NDSEM)] for e in ENG}
        final = {e: {} for e in ENG}
        extra_sems = []
        for e in ENG:
            cnt = 0
            nd = 0
            uses = [0] * NDSEM
            for o in self.ops[e]:
                if o.is_dma:
                    k = nd % NDSEM
                    nd += 1
                    uses[k] += 1
                    o.tok = (dsem[e][k], 16 * uses[k])
                    o.prev = (dsem[e][k], 16 * (uses[k] - 1))
                    final[e][k] = o.tok
                elif o.own:
                    o.tok = (nc.alloc_semaphore("own_%s%d" % (e, o.idx)), 1)
                    extra_sems.append(o.tok[0])
                elif o.need_inc:
                    cnt += 1
                    o.tok = (esem[e], cnt)
        engobj = {"pe": "tensor", "act": "scalar", "dve": "vector", "pool": "gpsimd", "sp": "sync"}

        def emit_engine(e, eng):
            waited = {}

            def wait(tok):
                sem, val = tok
                if val <= 0:
                    return
                if waited.get(sem.num, 0) < val:
                    eng.wait_ge(sem, val)
                    waited[sem.num] = val

            for o in self.ops[e]:
                for d in o.deps:
                    wait(d.tok)
                if o.is_dma:
                    wait(o.prev)
                inst = o.fn(eng)
                if o.is_dma:
                    inst.then_inc(o.tok[0], 16)
                elif o.own:
                    inst.then_inc(o.tok[0])
                    final[e]["own%d" % o.idx] = o.tok
                elif o.need_inc:
                    inst.then_inc(o.tok[0], 1)
            for k, tok in final[e].items():
                wait(tok)

        allsems = list(esem.values()) + [x for e in ENG for x in dsem[e]] + extra_sems
        for sm_ in allsems:
            nc.gpsimd.sem_clear(sm_)
        nc.all_engine_barrier()
        with nc.Block() as blk:
            for e in ENG:
                if not self.ops[e]:
                    continue
                getattr(blk, engobj[e])(lambda eng, e=e: emit_engine(e, eng))
        for sm_ in allsems:
            nc.gpsimd.sem_clear(sm_)
        nc.all_engine_barrier()


class Arena:
    def __init__(self, nc, nbytes):
        self.nbytes = nbytes
        self.t = nc.alloc_sbuf_tensor("arena", [128, nbytes // 4], F32)
        self.views = {F32: self.t, BF16: self.t.bitcast(BF16), U32: self.t.bitcast(U32),
                      I32: self.t.bitcast(I32)}
        self.off = 0
        self.hi = 0

    def alloc(self, name, free_shape, dt, alias=(), at=None):
        esz = 2 if dt == BF16 else 4
        n = 1
        for s_ in free_shape:
            n *= s_
        nb = (n * esz + 31) // 32 * 32
        if at is None:
            at = self.off
            self.off += nb
        assert at % 32 == 0
        self.hi = max(self.hi, at + nb, self.off)
        assert self.hi <= self.nbytes, (name, self.hi, self.nbytes)
        v = self.views[dt][:, at // esz: at // esz + n]
        if len(free_shape) == 2:
            v = v.rearrange("p (a b) -> p a b", a=free_shape[0])
        elif len(free_shape) == 3:
            v = v.rearrange("p (a b c) -> p a b c", a=free_shape[0], b=free_shape[1])
        elif len(free_shape) == 4:
            v = v.rearrange("p (a b c d) -> p a b c d", a=free_shape[0], b=free_shape[1], c=free_shape[2])
        return Tl(v, Buf(name, alias))


def build(dbg=None, stop_after=None):
    nc = bass.Bass("TRN2", target_bir_lowering=False)
    P = Prog(nc)

    def din(name, shape, dt=F32):
        return nc.dram_tensor(name, list(shape), dt, kind="ExternalInput").ap()

    x_d = din("x", [T, D])
    w_in_d = din("w_in", [D, INW])
    g_mix_d = din("g_mix", [128, 8])
    wsT_d = din("wsT", [128, 8 * 128])
    bs_d = din("bs", [128, 8])
    gv_d = din("gv_bc", [128, 512])
    ropeC_d = din("ropeC", [128, NT * 64])
    ropeS_d = din("ropeS", [128, NT * 64])
    gq_d = din("gq_bc", [128, 64])
    gqs_d = din("gqs_bc", [128, 64])
    gk_d = din("gk_bc", [128, 64])
    gks_d = din("gks_bc", [128, 64])
    ident_d = din("ident", [128, 128])
    w_out_d = din("w_out", [D, D])
    g_ab_d = din("g_ab", [128, 8])
    gffn_d = din("gffn_bc", [128, D])
    gfin_d = din("gfin_bc", [128, D])
    wr_d = din("w_router", [D, NE])
    NWM = 1 if SKIP_WEIGHTS else (48 if NO_CC else 6)
    wexp_d = din("w_exp", [NWM * D, D])
    out_d = nc.dram_tensor("out", [T, D], F32, kind="ExternalOutput").ap()
    x1_d = nc.dram_tensor("x1_scr", [T, D], F32).ap()
    h2_d = nc.dram_tensor("h2_scr", [T, D], BF16).ap()
    wsrc_d = nc.dram_tensor("wsrc_bf", [NWM * D, D], BF16).ap()
    wall_d = nc.dram_tensor("wall_bf", [NCORES * 6 * D, D], BF16).ap()
    dbg_d = {}
    if dbg:
        for k, (shp, dt) in dbg.items():
            dbg_d[k] = nc.dram_tensor("dbg_" + k, list(shp), dt, kind="ExternalOutput").ap()

    A = Arena(nc, 206 * 1024)
    ps = nc.alloc_psum_tensor("ps", [128, 4096], F32)
    ps_bf = ps.bitcast(BF16)
    pb = [Buf("psum%d" % i) for i in range(8)]

    def psf(b, n=512, off=0):
        return ps[:, b * 512 + off: b * 512 + off + n]

    def psb(b, n=1024, off=0):
        return ps_bf[:, b * 1024 + off: b * 1024 + off + n]

    dram_x1 = [Buf("x1d%d" % s_) for s_ in range(NSEQ)]
    dram_h2 = [Buf("h2d%d" % s_) for s_ in range(NSEQ)]
    dram_out = Buf("outd")
    dram_dbg = Buf("dbgd")

    def dma(q, out, in_, r=(), w=()):
        eng = {"sp": "sp", "pool": "pool", "act": "act"}[q]
        return P.op(eng, lambda e: e.dma_start(out=out, in_=in_), r=r, w=w, dma=True)

    def mm(out, lhsT, rhs, start, stop, r, w):
        return P.op("pe", lambda e: e.matmul(out, lhsT, rhs, start=start, stop=stop), r=r, w=w)

    def tr(out, in_, ident, r, w):
        return P.op("pe", lambda e: e.transpose(out, in_, ident), r=r, w=w)

    def act(out, in_, func, r, w, scale=None, bias=None, accum=None, eng="act"):
        kw = {}
        if scale is not None:
            kw["scale"] = scale
        if bias is not None:
            kw["bias"] = bias
        if accum is not None:
            kw["accum_out"] = accum
        return P.op("act", lambda e: e.activation(out, in_, func, **kw), r=r, w=w)

    def tt(eng, out, in0, in1, op, r, w):
        return P.op(eng, lambda e: e.tensor_tensor(out, in0, in1, op), r=r, w=w)

    def ts(eng, out, in0, s1, op0, r, w, s2=None, op1=None):
        if op1 is None:
            return P.op(eng, lambda e: e.tensor_scalar(out, in0, s1, None, op0), r=r, w=w)
        return P.op(eng, lambda e: e.tensor_scalar(out, in0, s1, s2, op0, op1), r=r, w=w)

    def cp(eng, out, in_, r, w):
        if eng == "act":
            return P.op("act", lambda e: e.copy(out, in_), r=r, w=w)
        return P.op(eng, lambda e: e.tensor_copy(out, in_), r=r, w=w)

    def red(out, in_, op, r, w):
        return P.op("dve", lambda e: e.tensor_reduce(out, in_, AX.X, op), r=r, w=w)

    def rstd_from_ss(ss, n, nm, cols=1):
        act(ss.ap, ss.ap, AF.Sqrt, r=[ss.buf, eps_t.buf], w=[ss.buf], scale=1.0 / n, bias=eps_t.ap[:, 0:1])
        P.op("dve", lambda e: e.reciprocal(ss.ap, ss.ap), r=[ss.buf], w=[ss.buf])

    eps_t = A.alloc("eps", [1], F32)
    P.op("dve", lambda e: e.memset(eps_t.ap, EPS), w=[eps_t.buf])
    ident = A.alloc("ident", [128], F32)
    dma("sp", ident.ap, ident_d, w=[ident.buf])
    identb = A.alloc("identb", [128], BF16)
    cp("dve", identb.ap, ident.ap, r=[ident.buf], w=[identb.buf])
    gmix = A.alloc("gmix", [8], F32)
    dma("sp", gmix.ap, g_mix_d, w=[gmix.buf])
    gab = A.alloc("gab", [8], F32)
    dma("sp", gab.ap, g_ab_d, w=[gab.buf])
    bs = A.alloc("bs", [8], F32)
    dma("sp", bs.ap, bs_d, w=[bs.buf])
    gv = A.alloc("gv", [512], F32)
    dma("sp", gv.ap, gv_d, w=[gv.buf])
    gffn = A.alloc("gffn", [D], F32)
    dma("sp", gffn.ap, gffn_d, w=[gffn.buf])
    gfin = A.alloc("gfin", [D], F32)
    dma("sp", gfin.ap, gfin_d, w=[gfin.buf])
    wr = A.alloc("wr", [8, NE], F32)
    dma("sp", wr.ap, wr_d.rearrange("(k p) e -> p k e", p=128), w=[wr.buf])
    wsT = A.alloc("wsT", [8, 128], BF16)
    dma("pool", wsT.ap, wsT_d.rearrange("p (h q) -> p h q", h=8), w=[wsT.buf])
    Cq = A.alloc("Cq", [NT, 64], F32)
    Sq = A.alloc("Sq", [NT, 64], F32)
    Ck = A.alloc("Ck", [NT, 64], F32)
    Sk = A.alloc("Sk", [NT, 64], F32)
    g4 = A.alloc("g4", [4, 64], F32)
    for i, gd in enumerate((gq_d, gqs_d, gk_d, gks_d)):
        dma("sp", g4.ap[:, i, :], gd, w=[g4.buf])
    dma("sp", Cq.ap, ropeC_d.rearrange("p (i d) -> p i d", i=NT), w=[Cq.buf])
    dma("sp", Sq.ap, ropeS_d.rearrange("p (i d) -> p i d", i=NT), w=[Sq.buf])
    for i, (dst, src, sc) in enumerate(((Ck, Cq, 1.0), (Sk, Sq, 1.0), (Cq, Cq, 0.125), (Sq, Sq, 0.125))):
        gi = {0: 2, 1: 3, 2: 0, 3: 1}[i]
        gb = g4.ap[:, gi:gi + 1, :].to_broadcast([128, NT, 64])
        P.op("dve", lambda e, dst=dst, src=src, gb=gb, sc=sc: e.scalar_tensor_tensor(
            dst.ap, src.ap, sc, gb, ALU.mult, ALU.mult), r=[src.buf, g4.buf], w=[dst.buf])
    wout = A.alloc("wout", [8, D], BF16)
    if stop_after == "c0":
        return _finish(nc, P, A, out_d, dram_out, dma)
    moe_base = A.off
    win = A.alloc("win", [8, INW], BF16)
    stage = [A.alloc("stage%d" % i, [INW], F32) for i in range(2)]
    for k in range(8):
        st = stage[k % 2]
        dma("sp", st.ap, w_in_d[k * 128:(k + 1) * 128, :], w=[st.buf])
        eng = "dve" if k % 2 == 0 else "pool"
        ts(eng, win.ap[:, k, :], st.ap, gmix.ap[:, k:k + 1], ALU.mult, r=[st.buf, gmix.buf], w=[win.buf])
    for k in range(8):
        st = stage[k % 2]
        dma("sp", st.ap[:, 0:D], w_out_d[k * 128:(k + 1) * 128, :], w=[st.buf])
        eng = "dve" if k % 2 == 0 else "pool"
        ts(eng, wout.ap[:, k, :], st.ap[:, 0:D], gab.ap[:, k:k + 1], ALU.mult, r=[st.buf, gab.buf], w=[wout.buf])

    if stop_after == "c1":
        return _finish(nc, P, A, out_d, dram_out, dma)
    qT = A.alloc("qT", [4, S], BF16)
    kT = A.alloc("kT", [2, S], BF16)
    Vaug = A.alloc("Vaug", [NT, 2, 66], BF16)
    mixTa = A.alloc("mixTa", [4, S], BF16)
    p1tmp_base = A.off
    xt = [A.alloc("xt%d" % i, [D], F32) for i in range(2)]
    junk = A.alloc("junk", [D], BF16)
    xT = A.alloc("xT", [8, 128], BF16)
    uv = A.alloc("uv", [D], F32)
    sq = A.alloc("sq", [D], F32)
    vn = A.alloc("vn", [512], BF16)
    Aa = A.alloc("Aa", [512], F32)
    An = A.alloc("An", [512], BF16)
    qk = A.alloc("qk", [640], F32)
    qk2 = A.alloc("qk2", [640], F32)
    qkb = A.alloc("qkb", [512 + 256], BF16)
    small = A.alloc("small", [32], F32)
    p1_end = A.off

    def sm(a, b):
        return small.ap[:, a:b]

    P.op("pool", lambda e: e.memset(Vaug.ap, 1.0), w=[Vaug.buf])

    def phase1_tile(s_, i):
        tok0 = s_ * S + i * 128
        x = xt[i % 2]
        dma("sp", x.ap, x_d[tok0:tok0 + 128, :], w=[x.buf])
        act(junk.ap, x.ap, AF.Square, r=[x.buf], w=[junk.buf, small.buf], accum=sm(0, 1))
        act(sm(0, 1), sm(0, 1), AF.Sqrt, r=[small.buf, eps_t.buf], w=[small.buf], scale=1.0 / D, bias=eps_t.ap[:, 0:1])
        P.op("dve", lambda e: e.reciprocal(sm(0, 1), sm(0, 1)), r=[small.buf], w=[small.buf])
        rstd = sm(0, 1)
        for k in range(8):
            tr(psf(0 + k // 4, 128, (k % 4) * 128), x.ap[:, k * 128:(k + 1) * 128], ident.ap,
               r=[x.buf, ident.buf], w=[pb[k // 4]])
        cp("dve", xT.ap[:, 0:4, :], psf(0).rearrange("p (a b) -> p a b", a=4), r=[pb[0]], w=[xT.buf])
        cp("act", xT.ap[:, 4:8, :], psf(1).rearrange("p (a b) -> p a b", a=4), r=[pb[1]], w=[xT.buf])
        for k in range(8):
            for n in range(4):
                n0 = n * 512
                nn = min(512, INW - n0)
                mm(psf(2 + n, nn), xT.ap[:, k, :], win.ap[:, k, n0:n0 + nn], start=(k == 0), stop=(k == 7),
                   r=[xT.buf, win.buf], w=[pb[2 + n]])
        if stop_after == "tA":
            return
        pu = ps[:, 2 * 512: 2 * 512 + 1024]
        if GELU_NATIVE:
            for b_ in range(2):
                P.op("act", lambda e, b_=b_: e.activation(uv.ap[:, b_ * 512:(b_ + 1) * 512], psf(2 + b_), AF.Gelu_apprx_tanh, scale=rstd),
                     r=[pb[2 + b_], small.buf], w=[uv.buf])
        else:
            for b_ in range(2):
                P.op("act", lambda e, b_=b_: e.activation(uv.ap[:, b_ * 512:(b_ + 1) * 512], psf(2 + b_), AF.Identity, scale=rstd),
                     r=[pb[2 + b_], small.buf], w=[uv.buf])
            act(sq.ap, uv.ap, AF.Square, r=[uv.buf], w=[sq.buf])
            ts("dve", sq.ap, sq.ap, 0.044715, ALU.mult, r=[sq.buf], w=[sq.buf], s2=1.0, op1=ALU.add)
            tt("dve", sq.ap, sq.ap, uv.ap, ALU.mult, r=[sq.buf, uv.buf], w=[sq.buf])
            act(sq.ap, sq.ap, AF.Sigmoid, r=[sq.buf], w=[sq.buf], scale=GELU_C)
            tt("dve", uv.ap, uv.ap, sq.ap, ALU.mult, r=[sq.buf, uv.buf], w=[uv.buf])
        if stop_after == "tB":
            return
        pq = ps[:, 4 * 512: 4 * 512 + 640]
        ts("dve", qk.ap[:, 0:512], psf(4), rstd, ALU.mult, r=[pb[4], small.buf], w=[qk.buf])
        ts("dve", qk.ap[:, 512:640], psf(5, 128), rstd, ALU.mult, r=[pb[5], small.buf], w=[qk.buf])
        P.op("act", lambda e: e.activation(Vaug.ap[:, i, :, 0:64], psf(5, 128, 128).rearrange("p (a b) -> p a b", a=2),
                                           AF.Identity, scale=rstd), r=[pb[5], small.buf], w=[Vaug.buf])
        v = uv.ap[:, 512:1024]
        tt("pool", sq.ap[:, 0:512], v, v, ALU.mult, r=[uv.buf], w=[sq.buf])
        red(sm(8, 16), sq.ap[:, 0:512].rearrange("p (h d) -> p h d", h=8), ALU.add, r=[sq.buf], w=[small.buf])
        act(sm(8, 16), sm(8, 16), AF.Sqrt, r=[small.buf, eps_t.buf], w=[small.buf], scale=1.0 / 64, bias=eps_t.ap[:, 0:1])
        P.op("dve", lambda e: e.reciprocal(sm(8, 16), sm(8, 16)), r=[small.buf], w=[small.buf])
        tt("dve", sq.ap[:, 0:512].rearrange("p (h d) -> p h d", h=8), v.rearrange("p (h d) -> p h d", h=8),
           sm(8, 16).unsqueeze(2).to_broadcast([128, 8, 64]), ALU.mult, r=[uv.buf, small.buf], w=[sq.buf])
        tt("pool", vn.ap, sq.ap[:, 0:512], gv.ap, ALU.mult, r=[sq.buf, gv.buf], w=[vn.buf])
        for h in range(8):
            mm(psf(6, 64, h * 64), wsT.ap[:, h, :], vn.ap[:, h * 64:(h + 1) * 64], start=True, stop=True,
               r=[wsT.buf, vn.buf], w=[pb[6]])
        tt("dve", Aa.ap.rearrange("p (h d) -> p h d", h=8), psf(6).rearrange("p (h d) -> p h d", h=8),
           bs.ap.unsqueeze(2).to_broadcast([128, 8, 64]), ALU.add, r=[pb[6], bs.buf], w=[Aa.buf])
        tt("dve", Aa.ap, Aa.ap, uv.ap[:, 0:512], ALU.mult, r=[Aa.buf, uv.buf], w=[Aa.buf])
        act(junk.ap[:, 0:512], Aa.ap, AF.Square, r=[Aa.buf], w=[junk.buf, small.buf], accum=sm(1, 2))
        act(sm(1, 2), sm(1, 2), AF.Sqrt, r=[small.buf, eps_t.buf], w=[small.buf], scale=1.0 / 512, bias=eps_t.ap[:, 0:1])
        P.op("dve", lambda e: e.reciprocal(sm(1, 2), sm(1, 2)), r=[small.buf], w=[small.buf])
        P.op("act", lambda e: e.activation(An.ap, Aa.ap, AF.Identity, scale=sm(1, 2)), r=[Aa.buf, small.buf], w=[An.buf])
        for c in range(4):
            tr(psb(7, 128, c * 128), An.ap[:, c * 128:(c + 1) * 128], identb.ap, r=[An.buf, identb.buf], w=[pb[7]])
        cp("dve", mixTa.ap[:, :, i * 128:(i + 1) * 128], psb(7, 512).rearrange("p (a b) -> p a b", a=4),
           r=[pb[7]], w=[mixTa.buf])
        if stop_after == "tC":
            return
        tt("pool", qk2.ap, qk.ap, qk.ap, ALU.mult, r=[qk.buf], w=[qk2.buf])
        red(sm(16, 26), qk2.ap.rearrange("p (h d) -> p h d", h=10), ALU.add, r=[qk2.buf], w=[small.buf])
        act(sm(16, 26), sm(16, 26), AF.Sqrt, r=[small.buf, eps_t.buf], w=[small.buf], scale=1.0 / 64, bias=eps_t.ap[:, 0:1])
        P.op("dve", lambda e: e.reciprocal(sm(16, 26), sm(16, 26)), r=[small.buf], w=[small.buf])
        tt("dve", qk.ap.rearrange("p (h d) -> p h d", h=10), qk.ap.rearrange("p (h d) -> p h d", h=10),
           sm(16, 26).unsqueeze(2).to_broadcast([128, 10, 64]), ALU.mult, r=[qk.buf, small.buf], w=[qk.buf])
        if stop_after == "tD":
            return
        for (lo, nh, Ct, St) in ((0, 8, Cq, Sq), (512, 2, Ck, Sk)):
            xv = qk.ap[:, lo:lo + nh * 64]
            x5 = xv.rearrange("p (h a b d) -> p h a b d", h=nh, a=2, b=2)
            o2 = qk2.ap[:, lo:lo + nh * 64]
            o5 = o2.rearrange("p (h a b d) -> p h a b d", h=nh, a=2, b=2)
            S5 = St.ap[:, i, :].rearrange("p (a b d) -> p a b d", a=2, b=2)
            for a_ in range(2):
                for b_ in range(2):
                    tt("pool", o5[:, :, a_, b_, :], x5[:, :, a_, 1 - b_, :],
                       S5[:, a_, b_, :].unsqueeze(1).to_broadcast([128, nh, 16]), ALU.mult,
                       r=[qk.buf, St.buf], w=[qk2.buf])
            x3 = xv.rearrange("p (h d) -> p h d", h=nh)
            tt("dve", x3, x3, Ct.ap[:, i:i + 1, :].to_broadcast([128, nh, 64]), ALU.mult,
               r=[qk.buf, Ct.buf], w=[qk.buf])
        tt("dve", qkb.ap[:, 0:512], qk.ap[:, 0:512], qk2.ap[:, 0:512], ALU.add, r=[qk.buf, qk2.buf], w=[qkb.buf])
        kd = qkb.ap[:, 512:768].rearrange("p (kv r d) -> p kv r d", kv=2, r=2)
        for r_ in range(2):
            tt("pool", kd[:, :, r_, :], qk.ap[:, 512:640].rearrange("p (kv d) -> p kv d", kv=2),
               qk2.ap[:, 512:640].rearrange("p (kv d) -> p kv d", kv=2), ALU.add,
               r=[qk.buf, qk2.buf], w=[qkb.buf])
        if stop_after == "tE":
            return
        for c in range(4):
            tr(psb(0, 128, c * 128), qkb.ap[:, c * 128:(c + 1) * 128], identb.ap, r=[qkb.buf, identb.buf], w=[pb[0]])
        for c in range(2):
            tr(psb(0, 128, 512 + c * 128), qkb.ap[:, 512 + c * 128:512 + (c + 1) * 128], identb.ap,
               r=[qkb.buf, identb.buf], w=[pb[0]])
        cp("dve", qT.ap[:, :, i * 128:(i + 1) * 128], psb(0, 512).rearrange("p (a b) -> p a b", a=4),
           r=[pb[0]], w=[qT.buf])
        cp("dve", kT.ap[:, :, i * 128:(i + 1) * 128], psb(0, 256, 512).rearrange("p (a b) -> p a b", a=2),
           r=[pb[0]], w=[kT.buf])
        if dbg and s_ == 0 and i == 1:
            if "uv" in dbg_d:
                dma("sp", dbg_d["uv"], uv.ap, r=[uv.buf], w=[dram_dbg])
            if "Aa" in dbg_d:
                dma("sp", dbg_d["Aa"], Aa.ap, r=[Aa.buf], w=[dram_dbg])
            if "qkb" in dbg_d:
                dma("sp", dbg_d["qkb"], qkb.ap, r=[qkb.buf], w=[dram_dbg])
            if "vn" in dbg_d:
                dma("sp", dbg_d["vn"], vn.ap, r=[vn.buf], w=[dram_dbg])

    wall_buf = Buf("wall")
    wsrc_buf = Buf("wsrc")
    ws_at = A.off
    wstage = [A.alloc("wstage%d" % i, [4, D], BF16) for i in range(2)]
    def stage_gen():
        for j in range(0 if SKIP_WEIGHTS else NWM):
            for hlf in range(2):
                rows = slice(j * D + hlf * 512, j * D + (hlf + 1) * 512)
                dma("pool", wstage[hlf].ap, wexp_d[rows, :].rearrange("(k p) n -> p k n", p=128), w=[wstage[hlf].buf])
                yield
                dma("sp", wsrc_d[rows, :].rearrange("(k p) n -> p k n", p=128), wstage[hlf].ap,
                    r=[wstage[hlf].buf], w=[wsrc_buf])
                yield
        if not NO_CC and not SKIP_WEIGHTS:
            P.op("pool", lambda e: e.collective_compute("AllGather", ALU.bypass, replica_groups=[list(range(NCORES))],
                                                        ins=[wsrc_d.opt()], outs=[wall_d.opt()]),
                 r=[wsrc_buf], w=[wall_buf], own=True)
        yield
    stager = stage_gen()
    if NO_CC:
        for _ in stager:
            pass
    PT = [Tl(A.views[BF16][:, (ws_at + i * 2048) // 2:(ws_at + i * 2048) // 2 + 1024], Buf("PT%d" % i)) for i in range(4)]
    Bo = Tl(A.views[F32][:, (ws_at + 8192) // 4:(ws_at + 8192) // 4 + 2048].rearrange("p (a b) -> p a b", a=4), Buf("Bo"))
    late_alias = (PT, Bo)
    OT = [A.alloc("OT%d" % i, [512], F32) for i in range(2)]
    aff64 = A.alloc("aff64", [64], F32)
    affT = A.alloc("affT", [S], F32)
    tkv = A.alloc("tkv", [CAP], F32)
    tki = A.alloc("tki", [CAP], U32)
    tkf = A.alloc("tkf", [CAP], F32)
    idxT = A.alloc("idxT", [NSEQ, 2, NE], I32)
    gT = A.alloc("gT", [NSEQ, 2, NE], F32)
    P.op("pool", lambda e: e.memset(aff64.ap, 0.0), w=[aff64.buf])

    def wmat_src(e, m):
        if NO_CC:
            base = (e * 3 + m) * D
            return wsrc_d[base:base + D, :], wsrc_buf
        base = ((e // 2) * 6 + (e % 2) * 3 + m) * D
        return wall_d[base:base + D, :], wall_buf

    def topk_rounds(s_):
        rows = slice(s_ * 32, s_ * 32 + NE)
        R = affT.ap[rows, :]
        for r_ in range(CAP // 8):
            v8 = tkv.ap[rows, r_ * 8:(r_ + 1) * 8]
            i8 = tki.ap[rows, r_ * 8:(r_ + 1) * 8]
            P.op("dve", lambda e, v8=v8: e.max(v8, R), r=[affT.buf], w=[tkv.buf])
            P.op("dve", lambda e, v8=v8, i8=i8: e.max_index(i8, v8, R), r=[affT.buf, tkv.buf], w=[tki.buf])
            P.op("dve", lambda e, v8=v8: e.match_replace(R, v8, R, -1.0), r=[tkv.buf, affT.buf], w=[affT.buf])
            yield
        ts("dve", tkf.ap[rows, :], tki.ap[rows, :], float(s_ * S), ALU.add, r=[tki.buf], w=[tkf.buf])
        idn = ident.ap[rows, s_ * 32: s_ * 32 + NE]
        for hf in range(2):
            tr(psf(6, NE, hf * 32), tkf.ap[rows, hf * 128:(hf + 1) * 128], idn, r=[tkf.buf, ident.buf], w=[pb[6]])
            tr(psf(6, NE, 64 + hf * 32), tkv.ap[rows, hf * 128:(hf + 1) * 128], idn, r=[tkv.buf, ident.buf], w=[pb[6]])
        for hf in range(2):
            cp("dve", idxT.ap[:, s_, hf, :], psf(6, NE, hf * 32), r=[pb[6]], w=[idxT.buf])
            cp("dve", gT.ap[:, s_, hf, :], psf(6, NE, 64 + hf * 32), r=[pb[6]], w=[gT.buf])
        yield

    def drain(gen):
        if gen is not None:
            for _ in gen:
                pass

    def step(gen):
        if gen is not None:
            try:
                next(gen)
            except StopIteration:
                pass

    def phase3_tile(s_, i, t):
        tok0 = s_ * S + i * 128
        Bt = Bo.ap[:, t, :]
        act(junk.ap[:, 0:512], Bt, AF.Square, r=[Bo.buf], w=[junk.buf, small.buf], accum=sm(2, 3))
        act(sm(2, 3), sm(2, 3), AF.Sqrt, r=[small.buf, eps_t.buf], w=[small.buf], scale=1.0 / 512, bias=eps_t.ap[:, 0:1])
        P.op("dve", lambda e: e.reciprocal(sm(2, 3), sm(2, 3)), r=[small.buf], w=[small.buf])
        P.op("act", lambda e: e.activation(An.ap, Bt, AF.Identity, scale=sm(2, 3)), r=[Bo.buf, small.buf], w=[An.buf])
        for c in range(4):
            tr(psb(6, 128, c * 128), An.ap[:, c * 128:(c + 1) * 128], identb.ap, r=[An.buf, identb.buf], w=[pb[6]])
        mTb = vn.ap.rearrange("p (a b) -> p a b", a=4)
        cp("dve", mTb, psb(6, 512).rearrange("p (a b) -> p a b", a=4), r=[pb[6]], w=[vn.buf])
        for k in range(8):
            if k < 4:
                lh, lb = mixTa.ap[:, k, i * 128:(i + 1) * 128], mixTa.buf
            else:
                lh, lb = mTb[:, k - 4, :], vn.buf
            for n in range(2):
                mm(psf(n), lh, wout.ap[:, k, n * 512:(n + 1) * 512], start=(k == 0), stop=(k == 7),
                   r=[lb, wout.buf], w=[pb[n]])
        xa, x1 = xt[0], xt[1]
        dma("sp", xa.ap, x_d[tok0:tok0 + 128, :], w=[xa.buf])
        for n in range(2):
            tt("dve", x1.ap[:, n * 512:(n + 1) * 512], xa.ap[:, n * 512:(n + 1) * 512], psf(n), ALU.add,
               r=[xa.buf, pb[n]], w=[x1.buf])
        dma("sp", x1_d[tok0:tok0 + 128, :], x1.ap, r=[x1.buf, dram_x1[s_]])
        act(junk.ap, x1.ap, AF.Square, r=[x1.buf], w=[junk.buf, small.buf], accum=sm(3, 4))
        act(sm(3, 4), sm(3, 4), AF.Sqrt, r=[small.buf, eps_t.buf], w=[small.buf], scale=1.0 / D, bias=eps_t.ap[:, 0:1])
        P.op("dve", lambda e: e.reciprocal(sm(3, 4), sm(3, 4)), r=[small.buf], w=[small.buf])
        P.op("dve", lambda e: e.scalar_tensor_tensor(uv.ap, x1.ap, sm(3, 4), gffn.ap, ALU.mult, ALU.mult),
             r=[x1.buf, small.buf, gffn.buf], w=[uv.buf])
        cp("act", junk.ap, uv.ap, r=[uv.buf], w=[junk.buf])
        dma("sp", h2_d[tok0:tok0 + 128, :], junk.ap, r=[junk.buf, dram_h2[s_]])
        for k in range(8):
            tr(psf(2 + k // 4, 128, (k % 4) * 128), uv.ap[:, k * 128:(k + 1) * 128], ident.ap,
               r=[uv.buf, ident.buf], w=[pb[2 + k // 4]])
        h2T = sq.ap.rearrange("p (a b) -> p a b", a=8)
        cp("dve", h2T[:, 0:4, :], psf(2).rearrange("p (a b) -> p a b", a=4), r=[pb[2]], w=[sq.buf])
        cp("act", h2T[:, 4:8, :], psf(3).rearrange("p (a b) -> p a b", a=4), r=[pb[3]], w=[sq.buf])
        lg = psf(6, NE, 256)
        for k in range(8):
            mm(lg, h2T[:, k, :], wr.ap[:, k, :], start=(k == 0), stop=(k == 7), r=[sq.buf, wr.buf], w=[pb[6]])
        P.op("dve", lambda e: e.tensor_reduce(sm(4, 5), lg, AX.X, ALU.max), r=[pb[6]], w=[small.buf])
        ts("dve", sm(4, 5), sm(4, 5), -1.0, ALU.mult, r=[small.buf], w=[small.buf])
        afs = aff64.ap[:, s_ * 32: s_ * 32 + NE]
        P.op("act", lambda e: e.activation(afs, lg, AF.Exp, bias=sm(4, 5), accum_out=sm(5, 6)),
             r=[pb[6], small.buf], w=[aff64.buf, small.buf])
        P.op("dve", lambda e: e.reciprocal(sm(5, 6), sm(5, 6)), r=[small.buf], w=[small.buf])
        ts("dve", afs, afs, sm(5, 6), ALU.mult, r=[aff64.buf, small.buf], w=[aff64.buf])
        tr(psf(6, 128, 384)[0:64, :], aff64.ap, ident.ap, r=[aff64.buf, ident.buf], w=[pb[6]])
        rows = slice(s_ * 32, s_ * 32 + NE)
        cp("dve", affT.ap[rows, i * 128:(i + 1) * 128], psf(6, 128, 384)[rows, :], r=[pb[6]], w=[affT.buf])

    def attn_head(s_, qb, h, prev_post=None):
        kv, half, c = h // 4, h % 2, h // 2
        prt = slice(half * 64, half * 64 + 64)
        qTs = qT.ap[prt, c, qb * 512:(qb + 1) * 512]
        ob = 4 + (h % 2)

        def qk(g):
            for j in range(2):
                kt = 2 * g + j
                bank = (g % 2) * 2 + j
                mm(psf(bank), kT.ap[prt, kv, kt * 128:(kt + 1) * 128], qTs, start=True, stop=True,
                   r=[kT.buf, qT.buf], w=[pb[bank]])

        def ex(g):
            b0 = (g % 2) * 2
            pt = PT[g % 4]
            for j in range(2):
                P.op("act", lambda e, j=j: e.activation(pt.ap[:, j * 512:(j + 1) * 512], psf(b0 + j), AF.Exp),
                     r=[pb[b0 + j]], w=[pt.buf])

        def pv(g):
            pt = PT[g % 4]
            for j in range(2):
                kt = 2 * g + j
                mm(psf(ob)[0:65, :], Vaug.ap[:, kt, kv, 0:65], pt.ap[:, j * 512:(j + 1) * 512],
                   start=(kt == 0), stop=(kt == NT - 1), r=[Vaug.buf, pt.buf], w=[pb[ob]])

        qk(0)
        for g in range(8):
            if g + 1 < 8:
                qk(g + 1)
            ex(g)
            pv(g)
            if g == 2 and prev_post is not None:
                attn_post(*prev_post)
        ot = OT[h % 2]
        cp("dve", ot.ap[0:65, :], psf(ob)[0:65, :], r=[pb[ob]], w=[ot.buf])

    def attn_post(s_, qb, h):
        ot = OT[h % 2]
        for t in range(4):
            tr(psf(6, 65, t * 128), ot.ap[0:65, t * 128:(t + 1) * 128], ident.ap[0:65, 0:65],
               r=[ot.buf, ident.buf], w=[pb[6]])
        for t in range(4):
            P.op("dve", lambda e, t=t: e.reciprocal(sm(24 + t, 25 + t), psf(6, 1, t * 128 + 64)), r=[pb[6]], w=[small.buf])
            ts("dve", Bo.ap[:, t, h * 64:(h + 1) * 64], psf(6, 64, t * 128), sm(24 + t, 25 + t), ALU.mult,
               r=[pb[6], small.buf], w=[Bo.buf])

    pending_topk = None
    for s_ in range(NSEQ):
        for i in range(NT):
            phase1_tile(s_, i)
            if s_ == 0:
                step(stager); step(stager); step(stager); step(stager)
            if stop_after in ("t1", "tA", "tB", "tC", "tD", "tE") and i == 1:
                break
        if stop_after in ("p1", "t1", "tA", "tB", "tC", "tD", "tE"):
            break
        if s_ == 0:
            drain(stager)
            for pt_ in PT:
                pt_.buf.r += wstage[0].buf.w + wstage[0].buf.r
            Bo.buf.r += wstage[1].buf.w + wstage[1].buf.r
        prev_post = None
        for qb in range(4):
            for h in range(8):
                attn_head(s_, qb, h, prev_post)
                prev_post = (s_, qb, h)
                step(pending_topk)
            attn_post(*prev_post)
            prev_post = None
            for t in range(4):
                phase3_tile(s_, qb * 4 + t, t)
            if stop_after == "q1":
                break
        if stop_after == "q1":
            break
        drain(pending_topk)
        pending_topk = topk_rounds(s_)
        if stop_after == "s1":
            break
    drain(pending_topk)
    if dbg:
        for nm, tl in (("qT", qT), ("kT", kT), ("Vaug", Vaug), ("mixTa", mixTa), ("Bo", Bo), ("affT", affT),
                       ("tkv", tkv), ("tki", tki), ("idxT", idxT), ("gT", gT)):
            if stop_after == "q1" and nm in ("affT", "tkv", "tki", "idxT", "gT"):
                continue
            if nm in dbg_d:
                if nm in ("affT", "tkv", "tki"):
                    dma("sp", dbg_d[nm][0:16, :], tl.ap[0:16, :], r=[tl.buf], w=[dram_dbg])
                elif nm in ("idxT", "gT"):
                    dma("sp", dbg_d[nm][:, 0:32], tl.ap[:, 0, :, :], r=[tl.buf], w=[dram_dbg])
                else:
                    dma("sp", dbg_d[nm], tl.ap, r=[tl.buf], w=[dram_dbg])
        if "x1" in dbg_d:
            nrow = 512 if stop_after == "q1" else S
            dma("sp", dbg_d["x1"][0:nrow, :], x1_d[0:nrow, :], r=[dram_x1[0]], w=[dram_dbg])
    if stop_after is not None:
        return _finish(nc, P, A, out_d, dram_out, dma)

    P.op("dve", lambda e: e.memset(sm(30, 31), 0.0), w=[small.buf, dram_h2[0], dram_h2[1]])
    old = [win.buf, stage[0].buf, stage[1].buf, qT.buf, kT.buf, Vaug.buf, mixTa.buf]
    mo = moe_base
    wslot = []
    for i in range(3):
        wslot.append(A.alloc("wslot%d" % i, [8, D], BF16, alias=old if i == 0 else (), at=mo))
        mo += 16384
    if True:
        for wsl in wslot[1:]:
            wsl.buf.r += wslot[0].buf.r
    def malloc(name, shape, dt):
        nonlocal mo
        tl = A.alloc(name, shape, dt, at=mo)
        n = 1
        for q_ in shape:
            n *= q_
        mo += (n * (2 if dt == BF16 else 4) + 31) // 32 * 32
        tl.buf.r += wslot[0].buf.r
        return tl
    pre_moe_ops = list(wslot[0].buf.r)
    xs = malloc("xs", [4, D], BF16)
    xsT = malloc("xsT", [8, 512], BF16)
    hT = malloc("hT", [8, 512], BF16)
    sil = [malloc("sil%d" % i, [512], F32) for i in range(2)]
    yb = [malloc("yb%d" % i, [D], F32) for i in range(2)]
    assert mo <= p1tmp_base, (mo, p1tmp_base)

    nw = 0
    def load_w(e, m):
        nonlocal nw
        sl = wslot[nw % 3]
        nw += 1
        src, sbuf_ = wmat_src(e, m)
        for hlf in range(2):
            dma("sp", sl.ap[:, hlf * 4:(hlf + 1) * 4, :],
                src[hlf * 512:(hlf + 1) * 512, :].rearrange("(k p) n -> p k n", p=128), r=[sbuf_], w=[sl.buf])
        return sl

    def gather(e):
        for ct in range(4):
            s_, hf = ct // 2, ct % 2
            P.op("pool", lambda en, ct=ct, s_=s_, hf=hf, e=e: en.indirect_dma_start(
                out=xs.ap[:, ct, :], out_offset=None, in_=h2_d,
                in_offset=bass.IndirectOffsetOnAxis(ap=idxT.ap[:, s_, hf, e:e + 1], axis=0)),
                r=[idxT.buf, dram_h2[0], dram_h2[1]], w=[xs.buf], dma=True)

    gather(0)
    for e in range(NE):
        for ct in range(4):
            for k in range(8):
                tr(psb(ct, 128, k * 128), xs.ap[:, ct, k * 128:(k + 1) * 128], identb.ap,
                   r=[xs.buf, identb.buf], w=[pb[ct]])
            cp("dve", xsT.ap[:, :, ct * 128:(ct + 1) * 128],
               psb(ct, 1024).rearrange("p (k c) -> p k c", k=8), r=[pb[ct]], w=[xsT.buf])
        Wg = load_w(e, 0)
        Wu = load_w(e, 1)
        for ft in range(8):
            ba = 4 + (ft % 2) * 2
            bb = ba + 1
            for (bk, W_) in ((ba, Wg), (bb, Wu)):
                for k in range(8):
                    mm(psf(bk), W_.ap[:, k, ft * 128:(ft + 1) * 128], xsT.ap[:, k, :], start=(k == 0), stop=(k == 7),
                       r=[W_.buf, xsT.buf], w=[pb[bk]])
            sl_ = sil[ft % 2]
            act(sl_.ap, psf(ba), AF.Silu, r=[pb[ba]], w=[sl_.buf])
            tt("dve", hT.ap[:, ft, :], sl_.ap, psf(bb), ALU.mult, r=[sl_.buf, pb[bb]], w=[hT.buf])
        Wd = load_w(e, 2)
        if e + 1 < NE:
            gather(e + 1)
        for ct in range(4):
            s_, hf = ct // 2, ct % 2
            y = yb[ct % 2]
            gsc = gT.ap[:, s_, hf, e:e + 1]
            for dh in range(2):
                bk = (ct * 2 + dh) % 4
                for k in range(8):
                    mm(psf(bk), hT.ap[:, k, ct * 128:(ct + 1) * 128], Wd.ap[:, k, dh * 512:(dh + 1) * 512],
                       start=(k == 0), stop=(k == 7), r=[hT.buf, Wd.buf], w=[pb[bk]])
                if dh == 0:
                    P.op("act", lambda en, y=y, bk=bk, gsc=gsc: en.activation(y.ap[:, 0:512], psf(bk), AF.Identity, scale=gsc),
                         r=[pb[bk], gT.buf], w=[y.buf])
                else:
                    ts("dve", y.ap[:, 512:1024], psf(bk), gsc, ALU.mult, r=[pb[bk], gT.buf], w=[y.buf])
            P.op("pool", lambda en, y=y, s_=s_, hf=hf, e=e: en.indirect_dma_start(
                out=x1_d, out_offset=bass.IndirectOffsetOnAxis(ap=idxT.ap[:, s_, hf, e:e + 1], axis=0),
                in_=y.ap, in_offset=None, compute_op=ALU.add),
                r=[y.buf, idxT.buf], w=[dram_x1[s_]], dma=True)

    for s_ in range(NSEQ):
        for i in range(NT):
            tok0 = s_ * S + i * 128
            xa = xt[i % 2]
            dma("sp", xa.ap, x1_d[tok0:tok0 + 128, :], r=[dram_x1[s_]], w=[xa.buf])
            act(junk.ap, xa.ap, AF.Square, r=[xa.buf], w=[junk.buf, small.buf], accum=sm(6, 7))
            act(sm(6, 7), sm(6, 7), AF.Sqrt, r=[small.buf, eps_t.buf], w=[small.buf], scale=1.0 / D, bias=eps_t.ap[:, 0:1])
            P.op("dve", lambda e: e.reciprocal(sm(6, 7), sm(6, 7)), r=[small.buf], w=[small.buf])
            ob_ = uv if i % 2 == 0 else sq
            c0 = 6 + 2 * (i % 2)
            P.op("dve", lambda e, xa=xa, ob_=ob_: e.scalar_tensor_tensor(ob_.ap, xa.ap, sm(6, 7), gfin.ap, ALU.mult, ALU.mult),
                 r=[xa.buf, small.buf, gfin.buf], w=[ob_.buf])
            dma("sp", out_d[tok0:tok0 + 128, :], ob_.ap, r=[ob_.buf])
    P.emit()
    return nc


def _finish(nc, P, A, out_d, dram_out, dma):
    z = A.alloc("z", [D], F32)
    P.op("dve", lambda e: e.memset(z.ap, 0.0), w=[z.buf])
    dma("sp", out_d[0:128, :], z.ap, r=[z.buf], w=[dram_out])
    P.emit()
    return nc


def _rope_tables():
    half = 16
    inv_freq = (np.float32(10000.0) ** (-np.arange(half, dtype=np.float32) / np.float32(half))).astype(np.float32)
    C = np.zeros((128, NT, 64), np.float32)
    Sg = np.zeros((128, NT, 64), np.float32)
    p = np.arange(128)
    for i in range(NT):
        t = 128 * i + p
        row = (t // 64).astype(np.float32)
        col = (t % 64).astype(np.float32)
        ar = (row[:, None] * inv_freq[None, :]).astype(np.float32)
        ac = (col[:, None] * inv_freq[None, :]).astype(np.float32)
        C[:, i, 0:16] = np.cos(ar); C[:, i, 16:32] = np.cos(ar)
        C[:, i, 32:48] = np.cos(ac); C[:, i, 48:64] = np.cos(ac)
        Sg[:, i, 0:16] = -np.sin(ar); Sg[:, i, 16:32] = np.sin(ar)
        Sg[:, i, 32:48] = -np.sin(ac); Sg[:, i, 48:64] = np.sin(ac)
    return C.reshape(128, NT * 64), Sg.reshape(128, NT * 64)


def _swap(g):
    g = np.asarray(g, np.float32).reshape(2, 2, 16)
    return g[:, ::-1, :].reshape(64)


def make_in_maps(inputs):
    f = lambda a: np.ascontiguousarray(np.asarray(a, dtype=np.float32))
    x = f(inputs["x"])
    C, Sg = _rope_tables()
    gq = f(inputs["q_norm_g"]).reshape(64)
    gk = f(inputs["k_norm_g"]).reshape(64)
    shared = {
        "w_in": f(inputs["w_in"])[0],
        "g_mix": f(f(inputs["norm_mix_g"]).reshape(8, 128).T),
        "wsT": f(f(inputs["gmlp_w_s"])[0].transpose(2, 0, 1).reshape(128, 8 * 128)),
        "bs": f(f(inputs["gmlp_b_s"])[0].T),
        "gv_bc": f(np.tile(f(inputs["gmlp_v_norm_g"]).reshape(1, 512), (128, 1))),
        "ropeC": C, "ropeS": Sg,
        "gq_bc": f(np.tile(gq[None], (128, 1))), "gqs_bc": f(np.tile(_swap(gq)[None], (128, 1))),
        "gk_bc": f(np.tile(gk[None], (128, 1))), "gks_bc": f(np.tile(_swap(gk)[None], (128, 1))),
        "ident": np.eye(128, dtype=np.float32),
        "w_out": f(inputs["w_out"])[0],
        "g_ab": f(np.concatenate([f(inputs["group_norm_a_g"]).reshape(-1), f(inputs["group_norm_b_g"]).reshape(-1)]).reshape(8, 128).T),
        "gffn_bc": f(np.tile(f(inputs["norm_ffn_g"]).reshape(1, D), (128, 1))),
        "gfin_bc": f(np.tile(f(inputs["final_norm_g"]).reshape(1, D), (128, 1))),
        "w_router": f(inputs["w_router"])[0],
    }
    wg = f(inputs["w_gate"])[0]; wu = f(inputs["w_up"])[0]; wd = f(inputs["w_down"])[0]
    maps = []
    for c in range(NCORES):
        m = dict(shared)
        m["x"] = np.ascontiguousarray(x[c * NSEQ:(c + 1) * NSEQ].reshape(T, D))
        el = range(NE) if NO_CC else (2 * c, 2 * c + 1)
        if SKIP_WEIGHTS:
            m["w_exp"] = np.zeros((D, D), np.float32)
            maps.append(m)
            continue
        m["w_exp"] = np.ascontiguousarray(np.concatenate([w[e] for e in el for w in (wg, wu, wd)], axis=0))
        maps.append(m)
    return maps


def kernel(**inputs):
    nc = build()
    maps = make_in_maps(inputs)
    res = run_bass_kernel_spmd(nc, maps, core_ids=list(range(NCORES)))
    out = np.stack([np.asarray(r["out"]).reshape(NSEQ, S, D) for r in res.results], axis=0)
    return out.reshape(NCORES * NSEQ, S, D).astype(np.float32)
```
